# Optimizing a Trainium2 kernel written in Bass

```python
import jax
import jax.numpy as jnp
from jax import lax
import numpy as np


D_MODEL = 1024
BATCH = 16
SEQ = 2048
DEPTH = 4

N_MIXERS = 2
N_MLA_LAYERS = (DEPTH + N_MIXERS - 1) // N_MIXERS
N_MLSTM_LAYERS = DEPTH // N_MIXERS

MLA_HEADS = D_MODEL // 128
MLA_NOPE_DIM = 128
MLA_ROPE_DIM = 64
MLA_V_DIM = 128
MLA_Q_RANK = 3 * D_MODEL // 8
MLA_KV_RANK = D_MODEL // 4
ROPE_BASE = 10000.0
Q_BLOCK = 128

MLSTM_HEADS = 4
MLSTM_V_DIM = D_MODEL // MLSTM_HEADS
MLSTM_QK_DIM = MLSTM_V_DIM // 2
MLSTM_CHUNK = 64

N_MEM = 256
MEM_HEADS = 4
MEM_HEAD_DIM = D_MODEL // MEM_HEADS

D_FF = -(-8 * D_MODEL // (3 * 256)) * 256

NORM_EPS = 1e-6

kernel_name = 'hybrid_mla_mlstm_memory_block'


def rmsnorm(x, g):
    xf = x.astype(jnp.float32)
    y = xf * lax.rsqrt(jnp.mean(xf * xf, axis=-1, keepdims=True) + NORM_EPS)
    return (y * g.astype(jnp.float32)).astype(x.dtype)


def rope_tables(positions):
    inv_freq = ROPE_BASE ** (-jnp.arange(0, MLA_ROPE_DIM, 2, dtype=jnp.float32) / MLA_ROPE_DIM)
    ang = positions.astype(jnp.float32)[..., None] * inv_freq
    return jnp.cos(ang), jnp.sin(ang)


def apply_rope(x, cos, sin):
    half = x.shape[-1] // 2
    x1, x2 = x[..., :half], x[..., half:]
    return jnp.concatenate([x1 * cos - x2 * sin, x2 * cos + x1 * sin], axis=-1).astype(x.dtype)


def mla_mixer(h, cos, sin, w_in, q_norm, w_uq, kv_norm, w_ukv, w_o):
    B, S, _ = h.shape
    H = MLA_HEADS
    proj = h @ w_in
    cq, ckv, k_rope = jnp.split(proj, [MLA_Q_RANK, MLA_Q_RANK + MLA_KV_RANK], axis=-1)
    q = (rmsnorm(cq, q_norm) @ w_uq).reshape(B, S, H, MLA_NOPE_DIM + MLA_ROPE_DIM)
    q_nope = q[..., :MLA_NOPE_DIM]
    q_rope = apply_rope(q[..., MLA_NOPE_DIM:], cos[:, :, None, :], sin[:, :, None, :])
    kv = (rmsnorm(ckv, kv_norm) @ w_ukv).reshape(B, S, H, MLA_NOPE_DIM + MLA_V_DIM)
    k_nope, v = kv[..., :MLA_NOPE_DIM], kv[..., MLA_NOPE_DIM:]
    k_rope = apply_rope(k_rope, cos, sin)
    scale = (MLA_NOPE_DIM + MLA_ROPE_DIM) ** -0.5
    nb = S // Q_BLOCK
    key_idx = jnp.arange(S)

    def to_blocks(t):
        return t.reshape(B, nb, Q_BLOCK, *t.shape[2:]).swapaxes(0, 1)

    def attend(args):
        qn, qr, q_idx = args
        s = (jnp.einsum('bqhd,bkhd->bhqk', qn, k_nope)
             + jnp.einsum('bqhr,bkr->bhqk', qr, k_rope)).astype(jnp.float32) * scale
        s = jnp.where(key_idx[None, :] <= q_idx[:, None], s, -jnp.inf)
        p = jax.nn.softmax(s, axis=-1).astype(v.dtype)
        return jnp.einsum('bhqk,bkhd->bqhd', p, v)

    o = lax.map(attend, (to_blocks(q_nope), to_blocks(q_rope), key_idx.reshape(nb, Q_BLOCK)))
    o = o.swapaxes(0, 1).reshape(B, S, H * MLA_V_DIM)
    return o @ w_o


def mlstm_mixer(h, w_in, b_gates, head_norm, w_o):
    B, S, _ = h.shape
    H, DK, DV, L = MLSTM_HEADS, MLSTM_QK_DIM, MLSTM_V_DIM, MLSTM_CHUNK
    f32 = jnp.float32
    proj = h @ w_in
    q, k, v, o_pre, gates = jnp.split(
        proj, [H * DK, 2 * H * DK, 2 * H * DK + H * DV, 2 * H * DK + 2 * H * DV], axis=-1)
    gates = gates.astype(f32) + b_gates.astype(f32)
    log_i = gates[..., :H]
    log_f = jax.nn.log_sigmoid(gates[..., H:])
    q = q.astype(f32).reshape(B, S, H, DK) * DK ** -0.5
    k = k.astype(f32).reshape(B, S, H, DK)
    v = v.astype(f32).reshape(B, S, H, DV)
    nc = S // L

    def chunk_vec(t):
        return t.reshape(B, nc, L, H, t.shape[-1]).transpose(1, 0, 3, 2, 4)

    def chunk_gate(t):
        return t.reshape(B, nc, L, H).transpose(1, 0, 3, 2)

    causal = jnp.tril(jnp.ones((L, L), dtype=bool))

    def step(carry, xs):
        C, n, m = carry
        qc, kc, vc, lic, lfc = xs
        b = jnp.cumsum(lfc, axis=-1)
        d_mat = jnp.where(causal, b[..., :, None] - b[..., None, :] + lic[..., None, :], -jnp.inf)
        g = b + m[..., None]
        mt = jnp.maximum(g, jnp.max(d_mat, axis=-1))
        w_intra = jnp.exp(d_mat - mt[..., None])
        w_inter = jnp.exp(g - mt)
        s = jnp.einsum('bhtd,bhsd->bhts', qc, kc) * w_intra
        num = (w_inter[..., None] * jnp.einsum('bhvd,bhtd->bhtv', C, qc)
               + jnp.einsum('bhts,bhsv->bhtv', s, vc))
        den = w_inter * jnp.einsum('bhd,bhtd->bht', n, qc) + jnp.sum(s, axis=-1)
        hc = num / jnp.maximum(jnp.abs(den), jnp.exp(-mt))[..., None]
        b_last = b[..., -1]
        m_new = mt[..., -1]
        w_k = jnp.exp(b_last[..., None] - b + lic - m_new[..., None])
        decay = jnp.exp(b_last + m - m_new)
        C = decay[..., None, None] * C + jnp.einsum('bhs,bhsv,bhsd->bhvd', w_k, vc, kc)
        n = decay[..., None] * n + jnp.einsum('bhs,bhsd->bhd', w_k, kc)
        return (C, n, m_new), hc

    init = (jnp.zeros((B, H, DV, DK), f32), jnp.zeros((B, H, DK), f32), jnp.zeros((B, H), f32))
    xs = (chunk_vec(q), chunk_vec(k), chunk_vec(v), chunk_gate(log_i), chunk_gate(log_f))
    _, hs = lax.scan(step, init, xs)
    hs = hs.transpose(1, 0, 3, 2, 4).reshape(B, S, H, DV)
    hs = rmsnorm(hs, head_norm)
    out = jax.nn.sigmoid(o_pre.astype(f32)).reshape(B, S, H, DV) * hs
    return out.reshape(B, S, H * DV).astype(h.dtype) @ w_o


def memory_xattn(h, mem_n, w_q, w_kv, w_o):
    B, S, _ = h.shape
    q = (h @ w_q).reshape(B, S, MEM_HEADS, MEM_HEAD_DIM)
    kv = (mem_n @ w_kv).reshape(B, mem_n.shape[1], 2, MEM_HEADS, MEM_HEAD_DIM)
    k, v = kv[:, :, 0], kv[:, :, 1]
    s = jnp.einsum('bqhd,bkhd->bhqk', q, k).astype(jnp.float32) * MEM_HEAD_DIM ** -0.5
    p = jax.nn.softmax(s, axis=-1).astype(v.dtype)
    o = jnp.einsum('bhqk,bkhd->bqhd', p, v).reshape(B, S, MEM_HEADS * MEM_HEAD_DIM)
    return o @ w_o


def swiglu(h, w_gate_up, w_down):
    gate, up = jnp.split(h @ w_gate_up, 2, axis=-1)
    return (jax.nn.silu(gate) * up) @ w_down


def _dense(key, shape, fan_in):
    return jax.random.normal(key, shape, jnp.float32) * fan_in ** -0.5


def _gain(key, shape):
    return 1.0 + 0.02 * jax.random.normal(key, shape, jnp.float32)


def setup_inputs(seed: int = 0) -> dict:
    key = jax.random.key(seed)
    ks = jax.random.split(key, 32)
    D = D_MODEL
    mla_in_cols = MLA_Q_RANK + MLA_KV_RANK + MLA_ROPE_DIM
    mlstm_in_cols = 2 * MLSTM_HEADS * MLSTM_QK_DIM + 2 * MLSTM_HEADS * MLSTM_V_DIM + 2 * MLSTM_HEADS
    x = jax.random.normal(ks[0], (BATCH, SEQ, D), jnp.float32)
    mem = jax.random.normal(ks[1], (BATCH, N_MEM, D), jnp.float32)
    offsets = jax.random.randint(ks[2], (BATCH, 1), 0, 4096, dtype=jnp.int32)
    positions = offsets + jnp.arange(SEQ, dtype=jnp.int32)[None, :]
    b_input = 0.1 * jax.random.normal(ks[3], (N_MLSTM_LAYERS, MLSTM_HEADS), jnp.float32)
    b_forget = (jnp.linspace(3.0, 6.0, MLSTM_HEADS, dtype=jnp.float32)[None, :]
                + 0.1 * jax.random.normal(ks[4], (N_MLSTM_LAYERS, MLSTM_HEADS), jnp.float32))
    return {
        'x': x,
        'mem': mem,
        'positions': positions,
        'mla_w_in': _dense(ks[5], (N_MLA_LAYERS, D, mla_in_cols), D),
        'mla_q_norm': _gain(ks[6], (N_MLA_LAYERS, MLA_Q_RANK)),
        'mla_w_uq': _dense(ks[7], (N_MLA_LAYERS, MLA_Q_RANK, MLA_HEADS * (MLA_NOPE_DIM + MLA_ROPE_DIM)), MLA_Q_RANK),
        'mla_kv_norm': _gain(ks[8], (N_MLA_LAYERS, MLA_KV_RANK)),
        'mla_w_ukv': _dense(ks[9], (N_MLA_LAYERS, MLA_KV_RANK, MLA_HEADS * (MLA_NOPE_DIM + MLA_V_DIM)), MLA_KV_RANK),
        'mla_w_o': _dense(ks[10], (N_MLA_LAYERS, MLA_HEADS * MLA_V_DIM, D), MLA_HEADS * MLA_V_DIM),
        'mlstm_w_in': _dense(ks[11], (N_MLSTM_LAYERS, D, mlstm_in_cols), D),
        'mlstm_b_gates': jnp.concatenate([b_input, b_forget], axis=-1),
        'mlstm_head_norm': _gain(ks[12], (N_MLSTM_LAYERS, MLSTM_HEADS, MLSTM_V_DIM)),
        'mlstm_w_o': _dense(ks[13], (N_MLSTM_LAYERS, MLSTM_HEADS * MLSTM_V_DIM, D), MLSTM_HEADS * MLSTM_V_DIM),
        'norm_mix_pre': _gain(ks[14], (DEPTH, D)),
        'norm_mix_post': _gain(ks[15], (DEPTH, D)),
        'norm_mem_q': _gain(ks[16], (DEPTH, D)),
        'norm_mem_kv': _gain(ks[17], (DEPTH, D)),
        'norm_mem_post': _gain(ks[18], (DEPTH, D)),
        'norm_ffn_pre': _gain(ks[19], (DEPTH, D)),
        'norm_ffn_post': _gain(ks[20], (DEPTH, D)),
        'mem_w_q': _dense(ks[21], (DEPTH, D, MEM_HEADS * MEM_HEAD_DIM), D),
        'mem_w_kv': _dense(ks[22], (DEPTH, D, 2 * MEM_HEADS * MEM_HEAD_DIM), D),
        'mem_w_o': _dense(ks[23], (DEPTH, MEM_HEADS * MEM_HEAD_DIM, D), MEM_HEADS * MEM_HEAD_DIM),
        'ffn_w_gate_up': _dense(ks[24], (DEPTH, D, 2 * D_FF), D),
        'ffn_w_down': _dense(ks[25], (DEPTH, D_FF, D), D_FF),
    }


def reference(x, mem, positions, mla_w_in, mla_q_norm, mla_w_uq, mla_kv_norm, mla_w_ukv, mla_w_o,
              mlstm_w_in, mlstm_b_gates, mlstm_head_norm, mlstm_w_o,
              norm_mix_pre, norm_mix_post, norm_mem_q, norm_mem_kv, norm_mem_post,
              norm_ffn_pre, norm_ffn_post, mem_w_q, mem_w_kv, mem_w_o, ffn_w_gate_up, ffn_w_down):
    cos, sin = rope_tables(positions)
    for i in range(DEPTH):
        j = i // N_MIXERS
        h = rmsnorm(x, norm_mix_pre[i])
        if i % N_MIXERS == 0:
            h = mla_mixer(h, cos, sin, mla_w_in[j], mla_q_norm[j], mla_w_uq[j],
                          mla_kv_norm[j], mla_w_ukv[j], mla_w_o[j])
        else:
            h = mlstm_mixer(h, mlstm_w_in[j], mlstm_b_gates[j], mlstm_head_norm[j], mlstm_w_o[j])
        x = x + rmsnorm(h, norm_mix_post[i])
        h = memory_xattn(rmsnorm(x, norm_mem_q[i]), rmsnorm(mem, norm_mem_kv[i]),
                         mem_w_q[i], mem_w_kv[i], mem_w_o[i])
        x = x + rmsnorm(h, norm_mem_post[i])
        h = swiglu(rmsnorm(x, norm_ffn_pre[i]), ffn_w_gate_up[i], ffn_w_down[i])
        x = x + rmsnorm(h, norm_ffn_post[i])
    return x
```

```python
import math
from contextlib import ExitStack

import numpy as np
import concourse.bass as bass
import concourse.mybir as mybir
from concourse.bass_utils import run_bass_kernel_spmd

F32 = mybir.dt.float32
BF16 = mybir.dt.bfloat16
I32 = mybir.dt.int32
AF = mybir.ActivationFunctionType
ALU = mybir.AluOpType
DT_SIZE = {F32: 4, BF16: 2, I32: 4}

NCORES = 8
DM = 1024
SEQ = 2048
NKC = 8
TG = 512
NG = 4
DEPTH = 4
NMEM = 256
DFF = 2816
EPS = 1e-6


class T:
    __slots__ = ("ap", "cells")

    def __init__(self, ap, cells):
        self.ap = ap
        self.cells = tuple(cells)


class Buf:
    def __init__(self, name, handle, nbytes, cell_bytes):
        self.name = name
        self.h = handle
        self.nbytes = nbytes
        self.cb = cell_bytes
        self._views = {F32: handle}

    def _h(self, dt):
        if dt not in self._views:
            self._views[dt] = self.h.bitcast(dt)
        return self._views[dt]

    def v(self, dt, off, n, p0=0, p1=128):
        sz = DT_SIZE[dt]
        lo = off * sz
        hi = (off + n) * sz
        assert 0 <= lo and hi <= self.nbytes, (self.name, off, n, dt)
        cells = [(self.name, c) for c in range(lo // self.cb, (hi - 1) // self.cb + 1)]
        return T(self._h(dt)[p0:p1, off:off + n], cells)


class Op:
    __slots__ = ("eng", "fn", "deps", "dma", "signal", "event", "waits", "idx")


class Prog:
    ENGS = ("pe", "act", "dve", "pool", "sp")
    SAME_ENGINE_SYNC = ("act", "dve", "pool")

    def __init__(self, n_dma_sems=24):
        self.ops = []
        self.last_writer = {}
        self.readers = {}
        self.n_dma_sems = n_dma_sems
        self.dma_rr = 0
        self.dma_sem_last = [None] * n_dma_sems
        self.dma_sem_count = [0] * n_dma_sems

    def add(self, eng, fn, reads=(), writes=(), dma=False):
        op = Op()
        op.eng = eng
        op.fn = fn
        op.dma = dma
        op.signal = False
        op.idx = len(self.ops)
        deps = set()
        rc = []
        for t in reads:
            rc.extend(t.cells)
        wc = []
        for t in writes:
            wc.extend(t.cells)
        for c in rc:
            w = self.last_writer.get(c)
            if w is not None:
                deps.add(w)
            if c[0].startswith("ps"):
                for e2, r in self.readers.get(c, {}).items():
                    if e2 != eng and not isinstance(r, list):
                        deps.add(r)
        for c in wc:
            w = self.last_writer.get(c)
            if w is not None:
                deps.add(w)
            for r in self.readers.get(c, {}).values():
                if isinstance(r, list):
                    deps.update(r)
                else:
                    deps.add(r)
        if dma:
            s = self.dma_rr
            self.dma_rr = (self.dma_rr + 1) % self.n_dma_sems
            prev = self.dma_sem_last[s]
            if prev is not None:
                deps.add(prev)
            self.dma_sem_last[s] = op.idx
            self.dma_sem_count[s] += 1
            op.event = (("dma", s), 16 * self.dma_sem_count[s])
        else:
            op.event = None
        deps.discard(op.idx)
        op.deps = deps
        self.ops.append(op)
        for c in wc:
            self.last_writer[c] = op.idx
            self.readers[c] = {}
        for c in rc:
            d = self.readers.setdefault(c, {})
            if dma:
                d.setdefault("dma", []).append(op.idx)
            else:
                d[eng] = op.idx
        return op

    def finalize(self):
        ops = self.ops
        for op in ops:
            for d in op.deps:
                p = ops[d]
                if p.dma:
                    continue
                if p.eng != op.eng or op.dma:
                    p.signal = True
                elif op.eng in self.SAME_ENGINE_SYNC:
                    p.signal = True
        cnt = {e: 0 for e in self.ENGS}
        for op in ops:
            if op.dma:
                continue
            if op.signal:
                cnt[op.eng] += 1
                op.event = (("eng", op.eng), cnt[op.eng])
        seen = {e: {} for e in self.ENGS}
        nw = 0
        for op in ops:
            need = {}
            for d in op.deps:
                p = ops[d]
                if (not p.dma) and p.eng == op.eng and (not op.dma) and op.eng not in self.SAME_ENGINE_SYNC:
                    continue
                k, v = p.event
                if need.get(k, 0) < v:
                    need[k] = v
            s = seen[op.eng]
            waits = []
            for k, v in need.items():
                if s.get(k, 0) < v:
                    s[k] = v
                    waits.append((k, v))
            op.waits = waits
            nw += len(waits)
        self.stats = dict(n_ops=len(ops), n_waits=nw, sig=dict(cnt))
        return self.stats

    def emit(self, nc):
        ops = self.ops
        with ExitStack() as st:
            sems = {}
            for e in self.ENGS:
                sems[("eng", e)] = st.enter_context(nc.semaphore("s_" + e))
            for i in range(self.n_dma_sems):
                sems[("dma", i)] = st.enter_context(nc.semaphore("d_%d" % i))
            block = st.enter_context(nc.Block())

            def run(eng_name):
                def body(eng):
                    for op in ops:
                        if op.eng != eng_name:
                            continue
                        for k, v in op.waits:
                            eng.wait_ge(sems[k], v)
                        ins = op.fn(eng) if op.fn is not None else None
                        if op.dma:
                            ins.then_inc(sems[op.event[0]], 16)
                        elif op.signal:
                            ins.then_inc(sems[op.event[0]], 1)
                return body

            block.tensor(run("pe"))
            block.scalar(run("act"))
            block.vector(run("dve"))
            block.gpsimd(run("pool"))
            block.sync(run("sp"))


class Filler:
    def __init__(self):
        self.queue = []
        self.active = []

    def add(self, genfn):
        self.queue.append(genfn)

    def step(self):
        for g in self.active[:]:
            try:
                next(g)
            except StopIteration:
                self.active.remove(g)
        if self.queue:
            g = self.queue.pop(0)()
            try:
                next(g)
                self.active.append(g)
            except StopIteration:
                pass

    def drain(self):
        while self.queue or self.active:
            self.step()


NORM6 = ("norm_mix_pre", "norm_mix_post", "norm_mem_q", "norm_mem_post", "norm_ffn_pre", "norm_ffn_post")


def _ct_cols():
    cols = {}
    c = 0
    for i in range(DEPTH):
        for n in NORM6:
            cols[(n, i)] = c
            c += 8
    for i in range(DEPTH):
        cols[("norm_mem_kv", i)] = c
        c += 8
    for j in range(2):
        cols[("mla_q_norm", j)] = c
        c += 3
        cols[("mla_kv_norm", j)] = c
        c += 2
    for j in range(2):
        cols[("mlstm_head_norm", j)] = c
        c += 8
        cols[("mlstm_b", j)] = c
        c += 2
    cols["invf"] = c
    c += 1
    cols["shift"] = c
    c += 1
    cols["sign"] = c
    c += 1
    cols["n"] = c
    return cols


CT = _ct_cols()
NCT = CT["n"]


class MK:
    def __init__(self, nseq=2, layers=(0, DEPTH), stop=None):
        self.nseq = nseq
        self.layers = layers
        self.stop = stop
        self.nc = bass.Bass("TRN2", target_bir_lowering=False)
        self.P = Prog()
        self.gi = 0
        self.gring = (5, 6, 7)
        self.sqi = 0
        self.pti = 0
        self.ring_next = 0
        self.cpi = 0

    def dram_in(self, name, shape, dt=F32):
        return self.nc.dram_tensor(name, list(shape), dt, kind="ExternalInput").ap()

    def declare(self):
        ns = self.nseq
        d = {}
        d["x"] = self.dram_in("x", [ns, SEQ, DM])
        d["mem"] = self.dram_in("mem", [ns, NMEM, DM])
        d["pos"] = self.dram_in("pos", [ns, SEQ], I32)
        d["ct"] = self.dram_in("ct", [128, NCT])
        d["ident"] = self.dram_in("ident", [128, 128])
        d["sel"] = self.dram_in("sel", [128, 512])
        d["mla_wa"] = self.dram_in("mla_wa", [2, 128, 3072])
        d["mla_wb"] = self.dram_in("mla_wb", [2, 128, 3072])
        d["mla_wh"] = self.dram_in("mla_wh", [2, 8, 128, 1280])
        d["mla_wo"] = self.dram_in("mla_wo", [2, 128, 8192])
        d["ml_wh"] = self.dram_in("ml_wh", [2, 4, 128, 6144])
        d["ml_wg"] = self.dram_in("ml_wg", [2, 128, 64])
        d["ml_wo"] = self.dram_in("ml_wo", [2, 128, 8192])
        d["mm_wq"] = self.dram_in("mm_wq", [4, 128, 8192])
        d["mm_wkv"] = self.dram_in("mm_wkv", [4, 4, 128, 4096])
        d["mm_wo"] = self.dram_in("mm_wo", [4, 128, 8192])
        d["ff_gu"] = self.dram_in("ff_gu", [4, 11, 128, 4096])
        d["ff_dn"] = self.dram_in("ff_dn", [4, 8, 128, 2816])
        self.d = d
        self.out = self.nc.dram_tensor("out", [ns, SEQ, DM], F32, kind="ExternalOutput").ap()

    def alloc(self, st):
        nc = self.nc

        def sb(name, nbytes, cb):
            h = st.enter_context(nc.sbuf_tensor(name, [128, nbytes // 4], F32))
            return Buf(name, h, nbytes, cb)

        self.XT = sb("XT", 65536, 2048)
        self.BIG = sb("BIG", 49152, 1024)
        self.PH = sb("PH", 16384, 1024)
        self.WR = sb("WR", 32768, 8192)
        self.AUX = sb("AUX", 12288, 2048)
        self.SQ = sb("SQ", 2048, 1024)
        self.R32 = sb("R32", 22528, 2048)
        self.PT = sb("PT", 4096, 1024)
        self.CTB = sb("CTB", NCT * 4, NCT * 4)
        self.IDB = sb("IDB", 512, 512)
        self.SELB = sb("SELB", 2048, 2048)
        self.ONB = sb("ONB", 256, 256)
        self.WGB = sb("WGB", 576, 576)
        self.WTB = sb("WTB", 256, 256)
        self.SMB = sb("SMB", 256, 256)
        self.PS = []
        for b in range(8):
            h = st.enter_context(nc.psum_tensor("ps%d" % b, [128, 512], F32))
            self.PS.append(Buf("ps%d" % b, h, 2048, 2048))
        self.ones = self.ONB.v(BF16, 0, 128)
        self.ident = self.IDB.v(F32, 0, 128)

    def mm(self, out, lhsT, rhs, start, stop):
        self.P.add("pe", lambda e: e.matmul(out.ap, lhsT=lhsT.ap, rhs=rhs.ap, start=start, stop=stop),
                   reads=[lhsT, rhs], writes=[out])

    def tr(self, out, in_, ident):
        self.P.add("pe", lambda e: e.transpose(out.ap, in_.ap, ident.ap), reads=[in_, ident], writes=[out])

    def act(self, out, in_, func, bias=None, scale=None, extra=()):
        kw = {}
        rd = [in_] + list(extra)
        if bias is not None:
            if isinstance(bias, T):
                kw["bias"] = bias.ap
                rd.append(bias)
            else:
                kw["bias"] = float(bias)
        if scale is not None:
            if isinstance(scale, T):
                kw["scale"] = scale.ap
                rd.append(scale)
            else:
                kw["scale"] = float(scale)
        self.P.add("act", lambda e: e.activation(out=out.ap, in_=in_.ap, func=func, **kw), reads=rd, writes=[out])

    def tt(self, out, in0, in1, op, eng="dve"):
        self.P.add(eng, lambda e: e.tensor_tensor(out=out.ap, in0=in0.ap, in1=in1.ap, op=op),
                   reads=[in0, in1], writes=[out])

    def ts(self, out, in0, s1, op0, s2=None, op1=None, eng="dve"):
        rd = [in0]
        a1 = s1
        if isinstance(s1, T):
            rd.append(s1)
            a1 = s1.ap
        a2 = s2
        if isinstance(s2, T):
            rd.append(s2)
            a2 = s2.ap
        if op1 is None:
            self.P.add(eng, lambda e: e.tensor_scalar(out=out.ap, in0=in0.ap, scalar1=a1, scalar2=None, op0=op0),
                       reads=rd, writes=[out])
        else:
            self.P.add(eng, lambda e: e.tensor_scalar(out=out.ap, in0=in0.ap, scalar1=a1, scalar2=a2, op0=op0, op1=op1),
                       reads=rd, writes=[out])

    def stt(self, out, in0, scalar, in1, op0, op1):
        rd = [in0, in1]
        a = scalar
        if isinstance(scalar, T):
            rd.append(scalar)
            a = scalar.ap
        self.P.add("dve", lambda e: e.scalar_tensor_tensor(out=out.ap, in0=in0.ap, scalar=a, in1=in1.ap, op0=op0, op1=op1),
                   reads=rd, writes=[out])

    def copy(self, out, in_, eng=None, scale=None):
        if eng is None:
            eng = ("act", "dve")[self.cpi % 2]
            self.cpi += 1
        if eng == "act":
            self.act(out, in_, AF.Copy, scale=scale)
        else:
            if scale is None:
                self.P.add("dve", lambda e: e.tensor_copy(out=out.ap, in_=in_.ap), reads=[in_], writes=[out])
            else:
                self.ts(out, in_, float(scale), ALU.mult)

    def recip(self, out, in_):
        self.act(out, in_, AF.Ln)
        self.act(out, out, AF.Exp, scale=-1.0)

    def dma(self, eng, out_ap, in_ap, reads=(), writes=()):
        self.P.add(eng, lambda e: e.dma_start(out=out_ap, in_=in_ap), reads=reads, writes=writes, dma=True)

    def gbank(self):
        ring = self.gring
        b = ring[self.gi % len(ring)]
        self.gi += 1
        return self.PS[b]

    def psv(self, bank, off=0, n=512, p0=0, p1=128):
        return bank.v(F32, off, n, p0, p1)

    def r32(self, slot, n=512, off=0, p0=0, p1=128):
        return self.R32.v(F32, slot * 512 + off, n, p0, p1)

    def ctv(self, col, p0=0, p1=128):
        return self.CTB.v(F32, col, 1, p0, p1)

    def ptbuf(self, n=512):
        i = self.pti % 4
        self.pti += 1
        return self.PT.v(BF16, i * 512, n)

    def wload(self, dram_ap, n, slot=None):
        nslots = (n + 4095) // 4096
        if slot is not None:
            self.ring_next = slot
        if nslots == 2 and self.ring_next % 2 == 1:
            self.ring_next += 1
        slot = self.ring_next % 4
        assert slot + nslots <= 4
        self.ring_next += nslots
        base = slot * 4096
        v = self.WR.v(BF16, base, n)
        self.dma("pool", v.ap, dram_ap, writes=[v])
        return base

    def wv(self, base, off, n, p0=0, p1=128):
        return self.WR.v(BF16, base + off, n, p0, p1)

    def norm_stats(self, srcs, dn, rslot, n=512):
        bank = self.gbank()
        ps = self.psv(bank, 0, n)
        for i, s in enumerate(srcs):
            sq = self.SQ.v(BF16, (self.sqi % 2) * 512, n)
            self.sqi += 1
            self.act(sq, s, AF.Square)
            self.mm(ps, self.ones, sq, start=(i == 0), stop=(i == len(srcs) - 1))
        r = self.r32(rslot, n)
        self.act(r, ps, AF.Ln, scale=1.0 / dn, bias=EPS)
        self.act(r, r, AF.Exp, scale=-0.5)
        return r

    def xt(self, kc, g, n=512, off=0):
        return self.XT.v(F32, kc * SEQ + g * TG + off, n)

    def pre_norm(self, gcol, g, dst, rslot=0):
        rstd = self.norm_stats([self.xt(kc, g) for kc in range(NKC)], DM, rslot)
        for kc in range(NKC):
            self.stt(dst(kc), self.xt(kc, g), self.ctv(gcol + kc), rstd, ALU.mult, ALU.mult)

    def proj_post_norm(self, groups, emit_chain, gcol, banks):
        pend = None
        bi = 0
        for m in range(NKC):
            for gi_, (g, y, sbk, rs) in enumerate(groups):
                bank = self.PS[banks[bi % len(banks)]]
                bi += 1
                ps = self.psv(bank)
                emit_chain(m, gi_, ps)
                if pend is not None:
                    self.mm(self.psv(self.PS[pend[0]]), self.ones, pend[1], start=(pend[2] == 0), stop=(pend[2] == NKC - 1))
                self.copy(y(m), ps, eng="dve")
                sq = self.SQ.v(BF16, (self.sqi % 2) * 512, 512)
                self.sqi += 1
                self.act(sq, y(m), AF.Square)
                pend = (sbk, sq, m)
                if len(groups) > 1 and gi_ == 0:
                    pass
        self.mm(self.psv(self.PS[pend[0]]), self.ones, pend[1], start=(pend[2] == 0), stop=(pend[2] == NKC - 1))
        for (g, y, sbk, rs) in groups:
            r = self.r32(rs)
            self.act(r, self.psv(self.PS[sbk]), AF.Ln, scale=1.0 / DM, bias=EPS)
            self.act(r, r, AF.Exp, scale=-0.5)
            for m in range(NKC):
                self.tt(y(m), y(m), r, ALU.mult, eng="pool")
                self.stt(self.xt(m, g), y(m), self.ctv(gcol + m), self.xt(m, g), ALU.mult, ALU.add)

    def yview_ph(self, m):
        return self.PH.v(F32, m * 512, 512)

    def setup_consts(self):
        d = self.d
        ct = self.CTB.v(F32, 0, NCT)
        self.dma("sp", ct.ap, d["ct"], writes=[ct])
        self.dma("sp", self.ident.ap, d["ident"], writes=[self.ident])
        sel = self.SELB.v(F32, 0, 512)
        self.dma("sp", sel.ap, d["sel"], writes=[sel])
        self.P.add("dve", lambda e: e.memset(self.ones.ap, 1.0), writes=[self.ones])

    def load_x(self, s):
        x = self.d["x"]
        for g in range(NG):
            stage = self.PH.v(F32, 0, 4096)
            src = x[s, g * TG:(g + 1) * TG, :].rearrange("(t p) d -> p t d", p=128)
            self.dma("sp", stage.ap.rearrange("p (t d) -> p t d", t=4), src, writes=[stage])
            for kc in range(NKC):
                bank = self.gbank()
                for t in range(4):
                    self.tr(self.psv(bank, t * 128, 128), self.PH.v(F32, t * DM + kc * 128, 128), self.ident)
                self.copy(self.xt(kc, g), self.psv(bank))

    def store_out(self, s):
        for g in range(NG):
            stage = self.PH.v(F32, 0, 4096)
            for t in range(4):
                for half in range(2):
                    bank = self.gbank()
                    for c in range(4):
                        kc = half * 4 + c
                        self.tr(self.psv(bank, c * 128, 128), self.xt(kc, g, 128, t * 128), self.ident)
                    self.copy(self.PH.v(F32, t * DM + half * 512, 512), self.psv(bank))
            dst = self.out[s, g * TG:(g + 1) * TG, :].rearrange("(t p) d -> p t d", p=128)
            self.dma("sp", dst, stage.ap.rearrange("p (t d) -> p t d", t=4), reads=[stage])
            self.out_stage = stage

    def rope_tables(self, s):
        pos = self.d["pos"]
        tab = self.AUX.v(F32, 0, SEQ)
        tmpi = self.PH.v(I32, 0, SEQ)
        tmpf = self.PH.v(F32, 0, SEQ)
        tmp2 = self.PH.v(F32, SEQ, SEQ)
        tmp2i = self.PH.v(I32, SEQ, SEQ)
        self.dma("sp", tmpi.ap, pos[s, :].partition_broadcast(128), writes=[tmpi])
        self.P.add("dve", lambda e: e.tensor_copy(out=tab.ap, in_=tmpi.ap), reads=[tmpi], writes=[tab])
        self.ts(tab, tab, self.ctv(CT["invf"]), ALU.mult, self.ctv(CT["shift"]), ALU.add)
        self.ts(tmp2i, tab, 1.0 / (2.0 * math.pi), ALU.mult)
        self.P.add("dve", lambda e: e.tensor_copy(out=tmpf.ap, in_=tmp2i.ap), reads=[tmp2i], writes=[tmpf])
        C1 = 6.28125
        C2 = 2.0 * math.pi - C1
        self.stt(tab, tmpf, -C1, tab, ALU.mult, ALU.add)
        self.stt(tab, tmpf, -C2, tab, ALU.mult, ALU.add)
        self.ts(tmp2, tab, math.pi, ALU.is_gt)
        self.stt(tab, tmp2, -2.0 * math.pi, tab, ALU.mult, ALU.add)
        self.ts(tmp2, tab, -math.pi, ALU.is_lt)
        self.stt(tab, tmp2, 2.0 * math.pi, tab, ALU.mult, ALU.add)
        self.ts(tab, tab, math.pi, ALU.min, -math.pi, ALU.max)
        self.act(tab, tab, AF.Sin, scale=self.ctv(CT["sign"]))
        self.tab = tab

    def rope_apply(self, out, ps_r, ps_s, tok0, n, tslot):
        cos = self.AUX.v(F32, tok0, n, 0, 64)
        sin = self.AUX.v(F32, tok0, n, 64, 128)
        t1 = self.r32(tslot, n, 0, 0, 64)
        t2 = self.r32(tslot + 1, n, 0, 0, 64)
        self.tt(t1, ps_r, cos, ALU.mult)
        self.tt(t2, ps_s, sin, ALU.mult)
        self.tt(out, t1, t2, ALU.add)

    def attention(self, g, q_views, k_views, exp_fn, v_mm, finish, filler=None):
        ntile = 4 * g + 4

        def rest(kt, ps, col0, n, j):
            pt = self.ptbuf(n)
            exp_fn(ps, pt, kt, col0, n)
            if j >= 0:
                blk = T(pt.ap[:, 0:128], pt.cells)
                self.P.add("pool", lambda e, blk=blk: e.affine_select(
                    out=blk.ap, in_=blk.ap, pattern=[[1, 128]], compare_op=ALU.is_ge, fill=0.0,
                    base=0, channel_multiplier=-1), reads=[blk], writes=[blk])
            v_mm(kt, pt, col0, n, kt == 0, kt == ntile - 1)
            if filler is not None:
                filler.step()

        pend = None
        for kt in range(ntile):
            j = kt - 4 * g
            col0 = 128 * j if j > 0 else 0
            n = TG - col0
            sb = self.PS[kt % 2]
            ps = self.psv(sb, col0, n)
            ks = k_views(kt)
            qs = q_views(col0, n)
            for i in range(len(ks)):
                self.mm(ps, ks[i], qs[i], start=(i == 0), stop=(i == len(ks) - 1))
            if pend is not None:
                rest(*pend)
            pend = (kt, ps, col0, n, j)
        rest(*pend)
        finish()

    def mla(self, s, i):
        j = i // 2
        d = self.d
        A32 = lambda ch, tok0, n, p0=0, p1=128: self.BIG.v(BF16, ch * SEQ + tok0, n, p0, p1)
        OB = lambda h, tok0, n: self.BIG.v(BF16, 16384 + h * 1024 + tok0, n)
        Qn = lambda off, n: self.PH.v(BF16, off, n)
        Qr = lambda off, n: self.PH.v(BF16, 1024 + off, n, 0, 64)
        Kn = lambda off, n: self.PH.v(BF16, 2048 + off, n)
        Vh = lambda kt: self.PH.v(BF16, 4096 + kt * 128, 128)
        self.rope_tables(s)
        wa = self.wload(d["mla_wa"][j], 3072)
        wb = self.wload(d["mla_wb"][j], 3072)
        gpre = CT[("norm_mix_pre", i)]
        gq = CT[("mla_q_norm", j)]
        gkv = CT[("mla_kv_norm", j)]
        hTb = lambda b: (lambda kc: self.BIG.v(BF16, 16384 + b * 4096 + kc * 512, 512))
        self.pre_norm(gpre, 0, hTb(0), 0)
        for g in range(NG):
            hT = hTb(g % 2)
            c32 = lambda m: self.PH.v(F32, m * 512, 512)
            for m in range(5):
                bank = self.gbank()
                ps = self.psv(bank)
                for kc in range(NKC):
                    w = self.wv(wa, kc * 384 + m * 128, 128) if m < 3 else self.wv(wb, kc * 384 + (m - 3) * 128, 128)
                    self.mm(ps, w, hT(kc), start=(kc == 0), stop=(kc == NKC - 1))
                self.copy(c32(m), ps, eng="dve")
            b1 = self.gbank()
            b2 = self.gbank()
            pr = self.psv(b1, 0, TG, 0, 64)
            psw = self.psv(b2, 0, TG, 0, 64)
            for kc in range(NKC):
                self.mm(pr, self.wv(wb, kc * 384 + 256, 64), hT(kc), start=(kc == 0), stop=(kc == NKC - 1))
            for kc in range(NKC):
                self.mm(psw, self.wv(wb, kc * 384 + 320, 64), hT(kc), start=(kc == 0), stop=(kc == NKC - 1))
            if g + 1 < NG:
                self.pre_norm(gpre, g + 1, hTb((g + 1) % 2), 4 if (g + 1) % 2 else 0)
            self.rope_apply(A32(5, g * TG, TG, 0, 64), pr, psw, g * TG, TG, 2)
            rq = self.norm_stats([c32(m) for m in range(3)], 384, 1)
            for m in range(3):
                self.stt(A32(m, g * TG, TG), c32(m), self.ctv(gq + m), rq, ALU.mult, ALU.mult)
            rk = self.norm_stats([c32(3 + m) for m in range(2)], 256, 1)
            for m in range(2):
                self.stt(A32(3 + m, g * TG, TG), c32(3 + m), self.ctv(gkv + m), rk, ALU.mult, ALU.mult)
        wo = None
        scale = 192.0 ** -0.5
        BS = [
            dict(Qn=lambda off, n: self.PH.v(BF16, off, n),
                 Qr=lambda off, n: self.PH.v(BF16, 1024 + off, n, 0, 64),
                 Kn=lambda off, n: self.PH.v(BF16, 2048 + off, n),
                 V=lambda off, n: self.PH.v(BF16, 4096 + off, n)),
            dict(Qn=lambda off, n: self.PH.v(BF16, 6144 + off, n),
                 Qr=lambda off, n: self.PH.v(BF16, 7168 + off, n, 0, 64),
                 Kn=lambda off, n: self.BIG.v(BF16, 6 * SEQ + off, n),
                 V=lambda off, n: self.BIG.v(BF16, 7 * SEQ + off, n)),
        ]

        def gen_items(half, h, bs):
            nk = (half + 1) * 1024
            whb = self.wload(d["mla_wh"][j, h], 1280)
            wq = lambda kc, off, n: self.wv(whb, kc * 256 + off, n)
            wk = lambda kc: self.wv(whb, 768 + kc * 128, 128)
            wvv = lambda kc: self.wv(whb, 1024 + kc * 128, 128)
            items = []

            def k_item(kg):
                def f():
                    bank = self.gbank()
                    ps = self.psv(bank)
                    for kc in range(2):
                        self.mm(ps, wk(kc), A32(3 + kc, kg * TG, TG), start=(kc == 0), stop=(kc == 1))
                    yield
                    self.copy(bs["Kn"](kg * TG, TG), ps)
                return f

            def v_item(kq):
                def f():
                    bank = self.gbank()
                    for t in range(4):
                        kt = kq * 4 + t
                        ps = self.psv(bank, t * 128, 128)
                        for kc in range(2):
                            self.mm(ps, A32(3 + kc, kt * 128, 128), wvv(kc), start=(kc == 0), stop=(kc == 1))
                    yield
                    self.copy(bs["V"](kq * 512, 512), self.psv(bank))
                return f

            def qn_item(g2):
                def f():
                    g = 2 * half + g2
                    bank = self.gbank()
                    ps = self.psv(bank)
                    for kc in range(3):
                        self.mm(ps, wq(kc, 0, 128), A32(kc, g * TG, TG), start=(kc == 0), stop=(kc == 2))
                    yield
                    self.copy(bs["Qn"](g2 * TG, TG), ps)
                return f

            def qr_item(g2):
                def f():
                    g = 2 * half + g2
                    b1 = self.gbank()
                    b2 = self.gbank()
                    pr = self.psv(b1, 0, TG, 0, 64)
                    psw = self.psv(b2, 0, TG, 0, 64)
                    for kc in range(3):
                        self.mm(pr, wq(kc, 128, 64), A32(kc, g * TG, TG), start=(kc == 0), stop=(kc == 2))
                    for kc in range(3):
                        self.mm(psw, wq(kc, 192, 64), A32(kc, g * TG, TG), start=(kc == 0), stop=(kc == 2))
                    yield
                    self.rope_apply(bs["Qr"](g2 * TG, TG), pr, psw, g * TG, TG, 2)
                return f

            items.append(qn_item(0))
            items.append(qr_item(0))
            for kg in range(nk // TG):
                items.append(k_item(kg))
            for kq in range(nk // TG):
                items.append(v_item(kq))
            items.append(qn_item(1))
            items.append(qr_item(1))
            return items

        blk = 0
        filler = Filler()
        for half in range(2):
            self.gring = (6, 7)
            for it in gen_items(half, 0, BS[0]):
                filler.add(it)
            filler.drain()
            for h in range(8):
                bs = BS[h % 2]
                if h + 1 < 8:
                    for it in gen_items(half, h + 1, BS[(h + 1) % 2]):
                        filler.add(it)
                if h == 6:
                    wo = self.wload(d["mla_wo"][j], 8192)
                for g2 in range(2):
                    g = 2 * half + g2
                    ob = self.PS[2 + (blk % 2)]
                    dn = self.PS[4 + (blk % 2)]
                    blk += 1

                    def q_views(col0, n, g2=g2, bs=bs):
                        return [bs["Qn"](g2 * TG + col0, n), bs["Qr"](g2 * TG + col0, n)]

                    def k_views(kt, bs=bs):
                        return [bs["Kn"](kt * 128, 128), A32(5, kt * 128, 128, 0, 64)]

                    def exp_fn(ps, pt, kt, col0, n):
                        self.act(pt, ps, AF.Exp, scale=scale)

                    def v_mm(kt, pt, col0, n, first, last, ob=ob, dn=dn, bs=bs):
                        self.mm(self.psv(ob, col0, n), bs["V"](kt * 128, 128), pt, start=first, stop=last)
                        self.mm(self.psv(dn, col0, n), self.ones, pt, start=first, stop=last)

                    def finish(ob=ob, dn=dn, g2=g2, h=h):
                        rec = self.r32(4 + (h % 2))
                        self.recip(rec, self.psv(dn))
                        self.tt(OB(h, g2 * TG, TG), self.psv(ob), rec, ALU.mult)

                    self.attention(g, q_views, k_views, exp_fn, v_mm, finish, filler)
                filler.drain()
            self.gring = (5, 6, 7)
            gpost = CT[("norm_mix_post", i)]
            for g2 in range(2):
                g = 2 * half + g2
                def chain(m, gi_, ps, g2=g2, wo=wo):
                    for h in range(8):
                        self.mm(ps, self.wv(wo, h * 1024 + m * 128, 128), OB(h, g2 * TG, TG), start=(h == 0), stop=(h == 7))

                self.proj_post_norm([(g, self.yview_ph, 4, 0)], chain, gpost, (5, 6, 7, 0, 1))

    def mlstm(self, s, i):
        j = i // 2
        d = self.d
        A32 = lambda kc, tok0, n: self.BIG.v(BF16, kc * SEQ + tok0, n)
        OB = lambda c, tok0, n: self.BIG.v(BF16, 16384 + c * 1024 + tok0, n)
        Qn = lambda off, n: self.PH.v(BF16, off, n)
        Kn = lambda off, n: self.PH.v(BF16, 1024 + off, n)
        Vh = lambda kt, c: self.PH.v(BF16, 3072 + kt * 256 + c * 128, 128)
        gpre = CT[("norm_mix_pre", i)]
        GW = lambda tok0, n: self.AUX.v(F32, tok0, n, 0, 4)
        GU = lambda tok0, n: self.AUX.v(F32, tok0, n, 32, 36)
        GM = lambda tok0, n: self.AUX.v(F32, tok0, n, 64, 68)
        LI = lambda tok0, n: self.PH.v(F32, tok0, n, 0, 4)
        LF = lambda tok0, n: self.PH.v(F32, SEQ + tok0, n, 0, 4)
        wg = self.WGB.v(BF16, 0, 64)
        self.dma("pool", wg.ap, d["ml_wg"][j], writes=[wg])
        bcol = CT[("mlstm_b", j)]
        for g in range(NG):
            self.pre_norm(gpre, g, lambda kc: A32(kc, g * TG, TG))
            bi = self.gbank()
            bf = self.gbank()
            pi = self.psv(bi, 0, TG, 0, 4)
            pf = self.psv(bf, 0, TG, 0, 4)
            for kc in range(NKC):
                self.mm(pi, self.WGB.v(BF16, kc * 8, 4), A32(kc, g * TG, TG), start=(kc == 0), stop=(kc == NKC - 1))
            for kc in range(NKC):
                self.mm(pf, self.WGB.v(BF16, kc * 8 + 4, 4), A32(kc, g * TG, TG), start=(kc == 0), stop=(kc == NKC - 1))
            self.act(LI(g * TG, TG), pi, AF.Identity, bias=self.ctv(bcol, 0, 4))
            self.act(LF(g * TG, TG), pf, AF.Identity, bias=self.ctv(bcol + 1, 0, 4))
        li = LI(0, SEQ)
        lf = LF(0, SEQ)
        gu = GU(0, SEQ)
        gw = GW(0, SEQ)
        gm = GM(0, SEQ)
        self.act(lf, lf, AF.Exp, scale=-1.0)
        self.act(lf, lf, AF.Ln, bias=1.0)
        self.ts(lf, lf, -1.0, ALU.mult)
        mtmp = GW(0, SEQ)
        self.P.add("dve", lambda e: e.tensor_tensor_scan(out=mtmp.ap, data0=lf.ap, data1=li.ap, initial=0.0,
                                                         op0=ALU.add, op1=ALU.max), reads=[lf, li], writes=[mtmp])
        self.P.add("dve", lambda e: e.tensor_copy(out=gm.ap, in_=mtmp.ap), reads=[mtmp], writes=[gm])
        one = self.SMB.v(F32, 0, 1, 0, 4)
        self.P.add("dve", lambda e: e.memset(one.ap, 1.0), writes=[one])
        gb = self.R32.v(F32, 0, SEQ, 0, 4)
        self.P.add("dve", lambda e: e.tensor_tensor_scan(out=gb.ap, data0=one.ap.to_broadcast([4, SEQ]), data1=lf.ap,
                                                         initial=0.0, op0=ALU.mult, op1=ALU.add),
                   reads=[lf, one], writes=[gb])
        self.tt(gu, gb, mtmp, ALU.subtract)
        self.tt(gw, li, gb, ALU.subtract)
        bank = self.gbank()
        for kt in range(16):
            self.mm(self.psv(bank, kt * 4, 4), GW(kt * 128, 128), self.IDB.v(F32, 0, 4, 0, 4), start=True, stop=True)
        wt = self.WTB.v(F32, 0, 64)
        self.copy(wt, self.psv(bank, 0, 64), eng="dve")
        wo = None
        hn = CT[("mlstm_head_norm", j)]
        fl = Filler()
        blk = 0

        def bcast_item(h, g, su, se):
            def f():
                b1 = self.gbank()
                self.mm(self.psv(b1), self.SELB.v(F32, h * 128, 128, 32, 36), GU(g * TG, TG), start=True, stop=True)
                b2 = self.gbank()
                self.mm(self.psv(b2), self.SELB.v(F32, h * 128, 128, 64, 68), GM(g * TG, TG), start=True, stop=True)
                yield
                self.copy(self.r32(su), self.psv(b1), eng="dve")
                self.act(self.r32(se), self.psv(b2), AF.Exp, scale=-1.0)
            return f

        whb_next = self.wload(d["ml_wh"][j, 0], 6144, slot=0)
        for half in range(2):
            nk = (half + 1) * 1024
            for h in range(4):
                whb = whb_next
                wcol = lambda kc, off, n, whb=whb: self.wv(whb, kc * 768 + off, n)

                def k_item(kg, wcol=wcol):
                    def f():
                        bank = self.gbank()
                        ps = self.psv(bank)
                        for kc in range(NKC):
                            self.mm(ps, wcol(kc, 128, 128), A32(kc, kg * TG, TG), start=(kc == 0), stop=(kc == NKC - 1))
                        yield
                        self.copy(Kn(kg * TG, TG), ps)
                    return f

                def v_item(kp, wcol=wcol):
                    def f():
                        bank = self.gbank()
                        for t in range(2):
                            kt = kp * 2 + t
                            ps = self.psv(bank, t * 256, 256)
                            for kc in range(NKC):
                                self.mm(ps, A32(kc, kt * 128, 128), wcol(kc, 256, 256), start=(kc == 0), stop=(kc == NKC - 1))
                        yield
                        self.copy(self.PH.v(BF16, 3072 + kp * 512, 512), self.psv(bank))
                    return f

                def q_item(g2, wcol=wcol, half=half):
                    def f():
                        g = 2 * half + g2
                        bank = self.gbank()
                        ps = self.psv(bank)
                        for kc in range(NKC):
                            self.mm(ps, wcol(kc, 0, 128), A32(kc, g * TG, TG), start=(kc == 0), stop=(kc == NKC - 1))
                        yield
                        self.copy(Qn(g2 * TG, TG), ps, scale=128.0 ** -0.5)
                    return f

                su0, se0 = (4, 8)[blk % 2], (3, 9)[blk % 2]
                epi = fl.queue
                fl.queue = []
                fl.add(bcast_item(h, 2 * half, su0, se0))
                fl.add(q_item(0))
                for kg in range(nk // TG):
                    fl.add(k_item(kg))
                for kp in range(nk // 256):
                    fl.add(v_item(kp))
                fl.add(q_item(1))
                genq = fl.queue
                merged = []
                for ii in range(max(len(genq), len(epi))):
                    if ii < len(genq):
                        merged.append(genq[ii])
                    if ii < len(epi):
                        merged.append(epi[ii])
                fl.queue = merged
                fl.drain()
                if h < 3:
                    whb_next = self.wload(d["ml_wh"][j, h + 1], 6144)
                else:
                    wo = self.wload(d["ml_wo"][j], 8192)
                for g2 in range(2):
                    g = 2 * half + g2
                    su, se = (4, 8)[blk % 2], (3, 9)[blk % 2]
                    blk += 1
                    ubc = self.r32(su)
                    enm = self.r32(se)
                    n0 = self.PS[2]
                    n1 = self.PS[3]
                    dn = self.PS[4]
                    if g2 == 0:
                        fl.add(bcast_item(h, g + 1, (4, 8)[blk % 2], (3, 9)[blk % 2]))

                    def q_views(col0, n, g2=g2):
                        return [Qn(g2 * TG + col0, n)]

                    def k_views(kt):
                        return [Kn(kt * 128, 128)]

                    def exp_fn(ps, pt, kt, col0, n, h=h, ubc=ubc):
                        dsl = 5 + (kt % 2)
                        dt_ = self.r32(dsl, n)
                        ub = T(ubc.ap[:, col0:col0 + n], ubc.cells)
                        self.act(dt_, ub, AF.Exp, bias=self.WTB.v(F32, kt * 4 + h, 1))
                        self.tt(pt, ps, dt_, ALU.mult)

                    def v_mm(kt, pt, col0, n, first, last):
                        self.mm(self.psv(n0, col0, n), Vh(kt, 0), pt, start=first, stop=last)
                        self.mm(self.psv(n1, col0, n), Vh(kt, 1), pt, start=first, stop=last)
                        self.mm(self.psv(dn, col0, n), self.ones, pt, start=first, stop=last)

                    def finish(h=h, g=g, g2=g2, enm=enm, whb=whb):
                        fl.drain()
                        rec = self.r32(2)
                        hs = [self.r32(0), self.r32(1)]
                        self.copy(hs[0], self.psv(n0), eng="act")
                        self.copy(hs[1], self.psv(n1), eng="dve")
                        self.act(rec, self.psv(dn), AF.Abs)
                        sqs = [self.SQ.v(BF16, c * 512, 512) for c in range(2)]

                        def norm_item():
                            self.tt(rec, rec, enm, ALU.max)
                            self.recip(rec, rec)
                            self.tt(hs[0], hs[0], rec, ALU.mult)
                            self.tt(hs[1], hs[1], rec, ALU.mult)
                            yield
                            for c in range(2):
                                self.act(sqs[c], hs[c], AF.Square)

                        def stats_item():
                            yield
                            bank = self.gbank()
                            ps = self.psv(bank)
                            for c in range(2):
                                self.mm(ps, self.ones, sqs[c], start=(c == 0), stop=(c == 1))
                            yield
                            rh = self.r32(2)
                            self.act(rh, ps, AF.Ln, scale=1.0 / 256, bias=EPS)
                            self.act(rh, rh, AF.Exp, scale=-0.5)

                        def og_item(c):
                            def f():
                                rh = self.r32(2)
                                bank = self.gbank()
                                ps = self.psv(bank)
                                for kc in range(NKC):
                                    self.mm(ps, self.wv(whb, kc * 768 + 512 + c * 128, 128), A32(kc, g * TG, TG),
                                            start=(kc == 0), stop=(kc == NKC - 1))
                                yield
                                e = self.r32(7 if c == 0 else 10)
                                self.act(e, ps, AF.Exp, scale=-1.0)
                                self.act(e, e, AF.Ln, bias=1.0)
                                self.act(e, e, AF.Exp, scale=-1.0)
                                yield
                                self.stt(hs[c], hs[c], self.ctv(hn + h * 2 + c), rh, ALU.mult, ALU.mult)
                                self.tt(OB(h * 2 + c, g2 * TG, TG), hs[c], e, ALU.mult)
                            return f

                        fl.add(norm_item)
                        fl.add(stats_item)
                        fl.add(og_item(0))
                        fl.add(og_item(1))

                    self.attention(g, q_views, k_views, exp_fn, v_mm, finish, fl)
            fl.drain()
            if half == 0:
                whb_next = self.wload(d["ml_wh"][j, 0], 6144)
            gpost = CT[("norm_mix_post", i)]
            for g2 in range(2):
                g = 2 * half + g2
                def chain(m, gi_, ps, g2=g2, wo=wo):
                    for kc in range(NKC):
                        self.mm(ps, self.wv(wo, kc * 1024 + m * 128, 128), OB(kc, g2 * TG, TG), start=(kc == 0), stop=(kc == NKC - 1))

                self.proj_post_norm([(g, self.yview_ph, 4, 0)], chain, gpost, (5, 6, 7, 0, 1))
            if half == 0:
                wo = None

    def memx(self, s, i):
        d = self.d
        KM = lambda m, off, n: self.AUX.v(BF16, m * 256 + off, n)
        VM = lambda kt, off, n: self.AUX.v(BF16, 2048 + kt * 1024 + off, n)
        MT = lambda kc, off, n: self.PH.v(BF16, 4096 + kc * 256 + off, n)
        gq = CT[("norm_mem_q", i)]
        gpost = CT[("norm_mem_post", i)]
        hTb = lambda b: (lambda kc: self.BIG.v(BF16, b * 12288 + kc * 512, 512))
        Qg = lambda m: self.BIG.v(BF16, 4096 + m * 512, 512)
        Og = lambda m: self.BIG.v(BF16, 8192 + m * 512, 512)
        wq = self.wload(d["mm_wq"][i], 8192, slot=0)

        def qproj(g):
            hT = hTb(g % 2)
            for m in range(NKC):
                bank = self.gbank()
                ps = self.psv(bank)
                for kc in range(NKC):
                    self.mm(ps, self.wv(wq, kc * 1024 + m * 128, 128), hT(kc), start=(kc == 0), stop=(kc == NKC - 1))
                self.copy(Qg(m), ps, eng="act")

        self.pre_norm(gq, 0, hTb(0), 0)
        qproj(0)
        mtok = self.PH.v(F32, 0, 2048)
        self.dma("sp", mtok.ap.rearrange("p (t d) -> p t d", t=2),
                 d["mem"][s].rearrange("(t p) d -> p t d", p=128), writes=[mtok])
        ss = self.SMB.v(F32, 8, 2)
        junk = self.r32(7)
        for t in range(2):
            src = self.PH.v(F32, t * 1024, 512)
            src2 = self.PH.v(F32, t * 1024 + 512, 512)
            s1 = self.SMB.v(F32, 16 + 2 * t, 1)
            s2 = self.SMB.v(F32, 17 + 2 * t, 1)
            self.P.add("act", lambda e, src=src, s1=s1: e.activation(out=junk.ap, in_=src.ap, func=AF.Square, accum_out=s1.ap),
                       reads=[src], writes=[junk, s1])
            self.P.add("act", lambda e, src2=src2, s2=s2: e.activation(out=junk.ap, in_=src2.ap, func=AF.Square, accum_out=s2.ap),
                       reads=[src2], writes=[junk, s2])
            self.tt(self.SMB.v(F32, 8 + t, 1), s1, s2, ALU.add)
        self.act(ss, ss, AF.Ln, scale=1.0 / DM, bias=EPS)
        self.act(ss, ss, AF.Exp, scale=-0.5)
        for t in range(2):
            mt_ = self.PH.v(F32, t * 1024, 1024)
            self.ts(mt_, mt_, self.SMB.v(F32, 8 + t, 1), ALU.mult)
        gkv = CT[("norm_mem_kv", i)]
        for kc in range(NKC):
            bank = self.gbank()
            for t in range(2):
                self.tr(self.psv(bank, t * 128, 128), self.PH.v(F32, t * 1024 + kc * 128, 128), self.ident)
            self.ts(MT(kc, 0, 256), self.psv(bank, 0, 256), self.ctv(gkv + kc), ALU.mult)
        for piece in range(4):
            wb = self.wload(d["mm_wkv"][i, piece], 4096, slot=2 + piece % 2)
            if piece < 2:
                for mm_ in range(4):
                    m = piece * 4 + mm_
                    bank = self.gbank()
                    ps = self.psv(bank, 0, 256)
                    for kc in range(NKC):
                        self.mm(ps, self.wv(wb, kc * 512 + mm_ * 128, 128), MT(kc, 0, 256), start=(kc == 0), stop=(kc == NKC - 1))
                    self.copy(KM(m, 0, 256), ps)
            else:
                nh = piece - 2
                for kt in range(2):
                    bank = self.gbank()
                    ps = self.psv(bank)
                    for kc in range(NKC):
                        self.mm(ps, MT(kc, kt * 128, 128), self.wv(wb, kc * 512, 512), start=(kc == 0), stop=(kc == NKC - 1))
                    self.copy(VM(kt, nh * 512, 512), ps)
        wo = self.wload(d["mm_wo"][i], 8192, slot=2)
        for g in range(NG):
            hT = hTb(g % 2)
            if g > 0:
                qproj(g)
            if g + 1 < NG:
                self.pre_norm(gq, g + 1, hTb((g + 1) % 2), (g + 1) % 2)
            def pv(h, pts):
                o0, o1, dn = (self.PS[2], self.PS[3], self.PS[4]) if h % 2 == 0 else (self.PS[5], self.PS[6], self.PS[7])
                for kt in range(2):
                    self.mm(self.psv(o0), VM(kt, h * 256, 128), pts[kt], start=(kt == 0), stop=(kt == 1))
                for kt in range(2):
                    self.mm(self.psv(o1), VM(kt, h * 256 + 128, 128), pts[kt], start=(kt == 0), stop=(kt == 1))
                for kt in range(2):
                    self.mm(self.psv(dn), self.ones, pts[kt], start=(kt == 0), stop=(kt == 1))
                rec = self.r32(4)
                self.recip(rec, self.psv(dn))
                self.tt(Og(2 * h), self.psv(o0), rec, ALU.mult)
                self.tt(Og(2 * h + 1), self.psv(o1), rec, ALU.mult)

            pend = None
            for h in range(4):
                pts = []
                for kt in range(2):
                    sb = self.PS[kt % 2]
                    ps = self.psv(sb)
                    for c in range(2):
                        self.mm(ps, KM(2 * h + c, kt * 128, 128), Qg(2 * h + c), start=(c == 0), stop=(c == 1))
                    pt = self.ptbuf()
                    self.act(pt, ps, AF.Exp, scale=1.0 / 16.0)
                    pts.append(pt)
                if pend is not None:
                    pv(*pend)
                pend = (h, pts)
            pv(*pend)

            def chain(m, gi_, ps):
                for kc in range(NKC):
                    self.mm(ps, self.wv(wo, kc * 1024 + m * 128, 128), Og(kc), start=(kc == 0), stop=(kc == NKC - 1))

            self.proj_post_norm([(g, self.yview_ph, 4, 7)], chain, gpost, (5, 6, 7))

    def ffn(self, s, i):
        d = self.d
        gpre = CT[("norm_ffn_pre", i)]
        gpost = CT[("norm_ffn_post", i)]
        hT = lambda kc, g2: self.PH.v(BF16, kc * 1024 + g2 * 512, 512)
        AT = lambda jj, g2: self.BIG.v(BF16, jj * 1024 + g2 * 512, 512)

        def y1(m):
            if m < 6:
                return self.AUX.v(F32, m * 512, 512)
            return self.BIG.v(F32, 11264 + (m - 6) * 512, 512)

        for tg in range(2):
            for g2 in range(2):
                self.pre_norm(gpre, 2 * tg + g2, lambda kc: hT(kc, g2))
            loads = {}
            for p in range(min(2, 11)):
                loads[p] = self.wload(d["ff_gu"][i, p], 4096)
            dn_loads = {}
            for p in range(11):
                if p + 2 < 11:
                    loads[p + 2] = self.wload(d["ff_gu"][i, p + 2], 4096)
                elif p + 2 - 11 < 8:
                    dn_loads[p + 2 - 11] = self.wload(d["ff_dn"][i, p + 2 - 11], 2816)
                wb = loads[p]
                for jj in range(2):
                    for g2 in range(2):
                        bg = self.PS[(self.gi) % 8]
                        self.gi += 1
                        bu = self.PS[(self.gi) % 8]
                        self.gi += 1
                        pg = self.psv(bg)
                        pu = self.psv(bu)
                        for kc in range(NKC):
                            self.mm(pg, self.wv(wb, kc * 512 + jj * 128, 128), hT(kc, g2), start=(kc == 0), stop=(kc == NKC - 1))
                        for kc in range(NKC):
                            self.mm(pu, self.wv(wb, kc * 512 + 256 + jj * 128, 128), hT(kc, g2), start=(kc == 0), stop=(kc == NKC - 1))
                        sg = self.r32(self.sqi % 8)
                        self.sqi += 1
                        self.act(sg, pg, AF.Silu)
                        self.tt(AT(2 * p + jj, g2), sg, pu, ALU.mult)
            def chain(m, gi_, ps, tg=tg):
                if gi_ == 0 and m + 2 < 8:
                    dn_loads[m + 2] = self.wload(d["ff_dn"][i, m + 2], 2816)
                wb = dn_loads[m]
                for jj in range(22):
                    self.mm(ps, self.wv(wb, jj * 128, 128), AT(jj, gi_), start=(jj == 0), stop=(jj == 21))

            self.proj_post_norm([(2 * tg, self.yview_ph, 4, 0), (2 * tg + 1, y1, 3, 1)], chain, gpost, (0, 1, 2, 5, 6, 7))

    def build(self):
        self.declare()
        with ExitStack() as st:
            self.alloc(st)
            self.setup_consts()
            done = False
            for s in range(self.nseq):
                self.load_x(s)
                for i in range(self.layers[0], self.layers[1]):
                    for sub in range(3):
                        if sub == 0:
                            if i % 2 == 0:
                                self.mla(s, i)
                            else:
                                self.mlstm(s, i)
                        elif sub == 1:
                            self.memx(s, i)
                        else:
                            self.ffn(s, i)
                        if self.stop is not None and (i, sub) == tuple(self.stop):
                            done = True
                            break
                    if done:
                        break
                done = False
                self.store_out(s)
            self.P.add("sp", None, writes=[self.out_stage])
            self.stats = self.P.finalize()
            self.P.emit(self.nc)
        return self.nc


def _kc(w):
    k, n = w.shape
    return np.ascontiguousarray(w.reshape(k // 128, 128, n).transpose(1, 0, 2)).reshape(128, (k // 128) * n)


def host_consts():
    ct = np.zeros((128, NCT), np.float32)
    inv_freq = (10000.0 ** (-np.arange(0, 64, 2, dtype=np.float32) / np.float32(64))).astype(np.float32)
    p = np.arange(128)
    ct[:, CT["invf"]] = inv_freq[p % 32]
    ct[:, CT["shift"]] = np.where(p < 64, np.float32(math.pi / 2), np.float32(0.0))
    ct[:, CT["sign"]] = np.where((p >= 64) & (p < 96), np.float32(-1.0), np.float32(1.0))
    ident = np.eye(128, dtype=np.float32)
    sel = np.zeros((128, 4, 128), np.float32)
    for q in range(4):
        for h in range(4):
            sel[q * 32 + h, h, :] = 1.0
    return ct, ident, sel.reshape(128, 512)


def host_layout(inp):
    f = lambda k: np.asarray(inp[k], dtype=np.float32)
    ct, ident, sel = host_consts()
    for i in range(DEPTH):
        for n in NORM6 + ("norm_mem_kv",):
            ct[:, CT[(n, i)]:CT[(n, i)] + 8] = f(n)[i].reshape(8, 128).T
    for j in range(2):
        ct[:, CT[("mla_q_norm", j)]:CT[("mla_q_norm", j)] + 3] = f("mla_q_norm")[j].reshape(3, 128).T
        ct[:, CT[("mla_kv_norm", j)]:CT[("mla_kv_norm", j)] + 2] = f("mla_kv_norm")[j].reshape(2, 128).T
        hn = f("mlstm_head_norm")[j]
        for h in range(4):
            ct[:, CT[("mlstm_head_norm", j)] + 2 * h:CT[("mlstm_head_norm", j)] + 2 * h + 2] = hn[h].reshape(2, 128).T
        bg = f("mlstm_b_gates")[j]
        ct[0:4, CT[("mlstm_b", j)]] = bg[0:4]
        ct[0:4, CT[("mlstm_b", j)] + 1] = bg[4:8]
    w = {"ct": ct, "ident": ident, "sel": sel}
    w_in = f("mla_w_in")
    w_uq = f("mla_w_uq")
    w_ukv = f("mla_w_ukv")
    w_o = f("mla_w_o")
    wa = np.zeros((2, 128, 3072), np.float32)
    wb = np.zeros((2, 128, 3072), np.float32)
    wh = np.zeros((2, 8, 128, 1280), np.float32)
    wo = np.zeros((2, 128, 8192), np.float32)
    for j in range(2):
        wa[j] = _kc(w_in[j][:, 0:384])
        kr = w_in[j][:, 640:704]
        wb[j] = _kc(np.concatenate([w_in[j][:, 384:640], kr, kr[:, 32:64], kr[:, 0:32]], axis=1))
        for h in range(8):
            qn = w_uq[j][:, h * 192:h * 192 + 128]
            qr = w_uq[j][:, h * 192 + 128:h * 192 + 192]
            wq = _kc(np.concatenate([qn, qr, qr[:, 32:64], qr[:, 0:32]], axis=1))
            wk = _kc(w_ukv[j][:, h * 256:h * 256 + 128])
            wv = _kc(w_ukv[j][:, h * 256 + 128:h * 256 + 256])
            wh[j, h] = np.concatenate([wq, wk, wv], axis=1)
        wo[j] = _kc(w_o[j])
    w.update(mla_wa=wa, mla_wb=wb, mla_wh=wh, mla_wo=wo)
    m_in = f("mlstm_w_in")
    m_o = f("mlstm_w_o")
    mh = np.zeros((2, 4, 128, 6144), np.float32)
    mg = np.zeros((2, 128, 64), np.float32)
    mo = np.zeros((2, 128, 8192), np.float32)
    for j in range(2):
        for h in range(4):
            cols = np.concatenate([m_in[j][:, h * 128:(h + 1) * 128],
                                   m_in[j][:, 512 + h * 128:512 + (h + 1) * 128],
                                   m_in[j][:, 1024 + h * 256:1024 + (h + 1) * 256],
                                   m_in[j][:, 2048 + h * 256:2048 + (h + 1) * 256]], axis=1)
            mh[j, h] = _kc(cols)
        mg[j] = _kc(m_in[j][:, 3072:3080])
        mo[j] = _kc(m_o[j])
    w.update(ml_wh=mh, ml_wg=mg, ml_wo=mo)
    wq_ = f("mem_w_q")
    wkv_ = f("mem_w_kv")
    wo_ = f("mem_w_o")
    mq = np.zeros((4, 128, 8192), np.float32)
    mkv = np.zeros((4, 4, 128, 4096), np.float32)
    mo2 = np.zeros((4, 128, 8192), np.float32)
    for i in range(DEPTH):
        mq[i] = _kc(wq_[i])
        for p in range(4):
            mkv[i, p] = _kc(wkv_[i][:, p * 512:(p + 1) * 512])
        mo2[i] = _kc(wo_[i])
    w.update(mm_wq=mq, mm_wkv=mkv, mm_wo=mo2)
    gu_ = f("ffn_w_gate_up")
    dn_ = f("ffn_w_down")
    gu = np.zeros((4, 11, 128, 4096), np.float32)
    dn = np.zeros((4, 8, 128, 2816), np.float32)
    for i in range(DEPTH):
        for p in range(11):
            gu[i, p] = _kc(np.concatenate([gu_[i][:, p * 256:(p + 1) * 256],
                                           gu_[i][:, DFF + p * 256:DFF + (p + 1) * 256]], axis=1))
        for m in range(8):
            dn[i, m] = _kc(dn_[i][:, m * 128:(m + 1) * 128])
    w.update(ff_gu=gu, ff_dn=dn)
    return w


_NC_CACHE = {}


def kernel(**inputs):
    w = host_layout(inputs)
    x = np.asarray(inputs["x"], np.float32)
    mem = np.asarray(inputs["mem"], np.float32)
    pos = np.asarray(inputs["positions"], np.int32)
    if "nc" not in _NC_CACHE:
        _NC_CACHE["nc"] = MK(nseq=2).build()
    nc = _NC_CACHE["nc"]
    in_maps = []
    for c in range(NCORES):
        m = dict(w)
        m["x"] = np.ascontiguousarray(x[2 * c:2 * c + 2])
        m["mem"] = np.ascontiguousarray(mem[2 * c:2 * c + 2])
        m["pos"] = np.ascontiguousarray(pos[2 * c:2 * c + 2])
        in_maps.append(m)
    res = run_bass_kernel_spmd(nc, in_maps, core_ids=list(range(NCORES)))
    return np.concatenate([r["out"] for r in res.results], axis=0)
```

```python
import math
from contextlib import ExitStack

import numpy as np
import concourse.bass as bass
import concourse.mybir as mybir
from concourse.bass_utils import run_bass_kernel_spmd

F32 = mybir.dt.float32
BF16 = mybir.dt.bfloat16
I32 = mybir.dt.int32
AF = mybir.ActivationFunctionType
ALU = mybir.AluOpType
DT_SIZE = {F32: 4, BF16: 2, I32: 4}

NCORES = 8
DM = 1024
SEQ = 2048
NKC = 8
TG = 512
NG = 4
DEPTH = 4
NMEM = 256
DFF = 2816
EPS = 1e-6


class T:
    __slots__ = ("ap", "cells")

    def __init__(self, ap, cells):
        self.ap = ap
        self.cells = tuple(cells)


class Buf:
    def __init__(self, name, handle, nbytes, cell_bytes):
        self.name = name
        self.h = handle
        self.nbytes = nbytes
        self.cb = cell_bytes
        self._views = {F32: handle}

    def _h(self, dt):
        if dt not in self._views:
            self._views[dt] = self.h.bitcast(dt)
        return self._views[dt]

    def v(self, dt, off, n, p0=0, p1=128):
        sz = DT_SIZE[dt]
        lo = off * sz
        hi = (off + n) * sz
        assert 0 <= lo and hi <= self.nbytes, (self.name, off, n, dt)
        cells = [(self.name, c) for c in range(lo // self.cb, (hi - 1) // self.cb + 1)]
        return T(self._h(dt)[p0:p1, off:off + n], cells)


class Op:
    __slots__ = ("eng", "fn", "deps", "dma", "signal", "event", "waits", "idx")


class Prog:
    ENGS = ("pe", "act", "dve", "pool", "sp")
    SAME_ENGINE_SYNC = ("act", "dve", "pool")

    def __init__(self, n_dma_sems=24):
        self.ops = []
        self.last_writer = {}
        self.readers = {}
        self.n_dma_sems = n_dma_sems
        self.dma_rr = 0
        self.dma_sem_last = [None] * n_dma_sems
        self.dma_sem_count = [0] * n_dma_sems

    def add(self, eng, fn, reads=(), writes=(), dma=False):
        op = Op()
        op.eng = eng
        op.fn = fn
        op.dma = dma
        op.signal = False
        op.idx = len(self.ops)
        deps = set()
        rc = []
        for t in reads:
            rc.extend(t.cells)
        wc = []
        for t in writes:
            wc.extend(t.cells)
        for c in rc:
            w = self.last_writer.get(c)
            if w is not None:
                deps.add(w)
            if c[0].startswith("ps"):
                for e2, r in self.readers.get(c, {}).items():
                    if e2 != eng and not isinstance(r, list):
                        deps.add(r)
        for c in wc:
            w = self.last_writer.get(c)
            if w is not None:
                deps.add(w)
            for r in self.readers.get(c, {}).values():
                if isinstance(r, list):
                    deps.update(r)
                else:
                    deps.add(r)
        if dma:
            s = self.dma_rr
            self.dma_rr = (self.dma_rr + 1) % self.n_dma_sems
            prev = self.dma_sem_last[s]
            if prev is not None:
                deps.add(prev)
            self.dma_sem_last[s] = op.idx
            self.dma_sem_count[s] += 1
            op.event = (("dma", s), 16 * self.dma_sem_count[s])
        else:
            op.event = None
        deps.discard(op.idx)
        op.deps = deps
        self.ops.append(op)
        for c in wc:
            self.last_writer[c] = op.idx
            self.readers[c] = {}
        for c in rc:
            d = self.readers.setdefault(c, {})
            if dma:
                d.setdefault("dma", []).append(op.idx)
            else:
                d[eng] = op.idx
        return op

    def finalize(self):
        ops = self.ops
        for op in ops:
            for d in op.deps:
                p = ops[d]
                if p.dma:
                    continue
                if p.eng != op.eng or op.dma:
                    p.signal = True
                elif op.eng in self.SAME_ENGINE_SYNC:
                    p.signal = True
        cnt = {e: 0 for e in self.ENGS}
        for op in ops:
            if op.dma:
                continue
            if op.signal:
                cnt[op.eng] += 1
                op.event = (("eng", op.eng), cnt[op.eng])
        seen = {e: {} for e in self.ENGS}
        nw = 0
        for op in ops:
            need = {}
            for d in op.deps:
                p = ops[d]
                if (not p.dma) and p.eng == op.eng and (not op.dma) and op.eng not in self.SAME_ENGINE_SYNC:
                    continue
                k, v = p.event
                if need.get(k, 0) < v:
                    need[k] = v
            s = seen[op.eng]
            waits = []
            for k, v in need.items():
                if s.get(k, 0) < v:
                    s[k] = v
                    waits.append((k, v))
            op.waits = waits
            nw += len(waits)
        self.stats = dict(n_ops=len(ops), n_waits=nw, sig=dict(cnt))
        return self.stats

    def emit(self, nc):
        ops = self.ops
        with ExitStack() as st:
            sems = {}
            for e in self.ENGS:
                sems[("eng", e)] = st.enter_context(nc.semaphore("s_" + e))
            for i in range(self.n_dma_sems):
                sems[("dma", i)] = st.enter_context(nc.semaphore("d_%d" % i))
            block = st.enter_context(nc.Block())

            def run(eng_name):
                def body(eng):
                    for op in ops:
                        if op.eng != eng_name:
                            continue
                        for k, v in op.waits:
                            eng.wait_ge(sems[k], v)
                        ins = op.fn(eng) if op.fn is not None else None
                        if op.dma:
                            ins.then_inc(sems[op.event[0]], 16)
                        elif op.signal:
                            ins.then_inc(sems[op.event[0]], 1)
                return body

            block.tensor(run("pe"))
            block.scalar(run("act"))
            block.vector(run("dve"))
            block.gpsimd(run("pool"))
            block.sync(run("sp"))


class Filler:
    def __init__(self):
        self.queue = []
        self.active = []

    def add(self, genfn):
        self.queue.append(genfn)

    def step(self):
        for g in self.active[:]:
            try:
                next(g)
            except StopIteration:
                self.active.remove(g)
        if self.queue:
            g = self.queue.pop(0)()
            try:
                next(g)
                self.active.append(g)
            except StopIteration:
                pass

    def drain(self):
        while self.queue or self.active:
            self.step()


NORM6 = ("norm_mix_pre", "norm_mix_post", "norm_mem_q", "norm_mem_post", "norm_ffn_pre", "norm_ffn_post")


def _ct_cols():
    cols = {}
    c = 0
    for i in range(DEPTH):
        for n in NORM6:
            cols[(n, i)] = c
            c += 8
    for i in range(DEPTH):
        cols[("norm_mem_kv", i)] = c
        c += 8
    for j in range(2):
        cols[("mla_q_norm", j)] = c
        c += 3
        cols[("mla_kv_norm", j)] = c
        c += 2
    for j in range(2):
        cols[("mlstm_head_norm", j)] = c
        c += 8
        cols[("mlstm_b", j)] = c
        c += 2
    cols["invf"] = c
    c += 1
    cols["shift"] = c
    c += 1
    cols["sign"] = c
    c += 1
    cols["n"] = c
    return cols


CT = _ct_cols()
NCT = CT["n"]


class MK:
    def __init__(self, nseq=2, layers=(0, DEPTH), stop=None):
        self.nseq = nseq
        self.layers = layers
        self.stop = stop
        self.nc = bass.Bass("TRN2", target_bir_lowering=False)
        self.P = Prog()
        self.gi = 0
        self.gring = (5, 6, 7)
        self.sqi = 0
        self.pti = 0
        self.ring_next = 0
        self.cpi = 0

    def dram_in(self, name, shape, dt=F32):
        return self.nc.dram_tensor(name, list(shape), dt, kind="ExternalInput").ap()

    def declare(self):
        ns = self.nseq
        d = {}
        d["x"] = self.dram_in("x", [ns, SEQ, DM])
        d["mem"] = self.dram_in("mem", [ns, NMEM, DM])
        d["pos"] = self.dram_in("pos", [ns, SEQ], I32)
        d["ct"] = self.dram_in("ct", [128, NCT])
        d["ident"] = self.dram_in("ident", [128, 128])
        d["sel"] = self.dram_in("sel", [128, 512])
        d["mla_wa"] = self.dram_in("mla_wa", [2, 128, 3072])
        d["mla_wb"] = self.dram_in("mla_wb", [2, 128, 3072])
        d["mla_wh"] = self.dram_in("mla_wh", [2, 8, 128, 1280])
        d["mla_wo"] = self.dram_in("mla_wo", [2, 128, 8192])
        d["ml_wh"] = self.dram_in("ml_wh", [2, 4, 128, 6144])
        d["ml_wg"] = self.dram_in("ml_wg", [2, 128, 64])
        d["ml_wo"] = self.dram_in("ml_wo", [2, 128, 8192])
        d["mm_wq"] = self.dram_in("mm_wq", [4, 128, 8192])
        d["mm_wkv"] = self.dram_in("mm_wkv", [4, 4, 128, 4096])
        d["mm_wo"] = self.dram_in("mm_wo", [4, 128, 8192])
        d["ff_gu"] = self.dram_in("ff_gu", [4, 11, 128, 4096])
        d["ff_dn"] = self.dram_in("ff_dn", [4, 8, 128, 2816])
        self.d = d
        self.out = self.nc.dram_tensor("out", [ns, SEQ, DM], F32, kind="ExternalOutput").ap()

    def alloc(self, st):
        nc = self.nc

        def sb(name, nbytes, cb):
            h = st.enter_context(nc.sbuf_tensor(name, [128, nbytes // 4], F32))
            return Buf(name, h, nbytes, cb)

        self.XT = sb("XT", 65536, 2048)
        self.BIG = sb("BIG", 49152, 1024)
        self.PH = sb("PH", 16384, 1024)
        self.WR = sb("WR", 32768, 8192)
        self.AUX = sb("AUX", 12288, 2048)
        self.SQ = sb("SQ", 2048, 1024)
        self.R32 = sb("R32", 22528, 2048)
        self.PT = sb("PT", 4096, 1024)
        self.CTB = sb("CTB", NCT * 4, NCT * 4)
        self.IDB = sb("IDB", 512, 512)
        self.SELB = sb("SELB", 2048, 2048)
        self.ONB = sb("ONB", 256, 256)
        self.WGB = sb("WGB", 576, 576)
        self.WTB = sb("WTB", 256, 256)
        self.SMB = sb("SMB", 256, 256)
        self.PS = []
        for b in range(8):
            h = st.enter_context(nc.psum_tensor("ps%d" % b, [128, 512], F32))
            self.PS.append(Buf("ps%d" % b, h, 2048, 2048))
        self.ones = self.ONB.v(BF16, 0, 128)
        self.ident = self.IDB.v(F32, 0, 128)

    def mm(self, out, lhsT, rhs, start, stop):
        self.P.add("pe", lambda e: e.matmul(out.ap, lhsT=lhsT.ap, rhs=rhs.ap, start=start, stop=stop),
                   reads=[lhsT, rhs], writes=[out])

    def tr(self, out, in_, ident):
        self.P.add("pe", lambda e: e.transpose(out.ap, in_.ap, ident.ap), reads=[in_, ident], writes=[out])

    def act(self, out, in_, func, bias=None, scale=None, extra=()):
        kw = {}
        rd = [in_] + list(extra)
        if bias is not None:
            if isinstance(bias, T):
                kw["bias"] = bias.ap
                rd.append(bias)
            else:
                kw["bias"] = float(bias)
        if scale is not None:
            if isinstance(scale, T):
                kw["scale"] = scale.ap
                rd.append(scale)
            else:
                kw["scale"] = float(scale)
        self.P.add("act", lambda e: e.activation(out=out.ap, in_=in_.ap, func=func, **kw), reads=rd, writes=[out])

    def tt(self, out, in0, in1, op, eng="dve"):
        self.P.add(eng, lambda e: e.tensor_tensor(out=out.ap, in0=in0.ap, in1=in1.ap, op=op),
                   reads=[in0, in1], writes=[out])

    def ts(self, out, in0, s1, op0, s2=None, op1=None, eng="dve"):
        rd = [in0]
        a1 = s1
        if isinstance(s1, T):
            rd.append(s1)
            a1 = s1.ap
        a2 = s2
        if isinstance(s2, T):
            rd.append(s2)
            a2 = s2.ap
        if op1 is None:
            self.P.add(eng, lambda e: e.tensor_scalar(out=out.ap, in0=in0.ap, scalar1=a1, scalar2=None, op0=op0),
                       reads=rd, writes=[out])
        else:
            self.P.add(eng, lambda e: e.tensor_scalar(out=out.ap, in0=in0.ap, scalar1=a1, scalar2=a2, op0=op0, op1=op1),
                       reads=rd, writes=[out])

    def stt(self, out, in0, scalar, in1, op0, op1):
        rd = [in0, in1]
        a = scalar
        if isinstance(scalar, T):
            rd.append(scalar)
            a = scalar.ap
        self.P.add("dve", lambda e: e.scalar_tensor_tensor(out=out.ap, in0=in0.ap, scalar=a, in1=in1.ap, op0=op0, op1=op1),
                   reads=rd, writes=[out])

    def copy(self, out, in_, eng=None, scale=None):
        if eng is None:
            eng = ("act", "dve")[self.cpi % 2]
            self.cpi += 1
        if eng == "act":
            self.act(out, in_, AF.Copy, scale=scale)
        else:
            if scale is None:
                self.P.add("dve", lambda e: e.tensor_copy(out=out.ap, in_=in_.ap), reads=[in_], writes=[out])
            else:
                self.ts(out, in_, float(scale), ALU.mult)

    def recip(self, out, in_):
        self.act(out, in_, AF.Ln)
        self.act(out, out, AF.Exp, scale=-1.0)

    def dma(self, eng, out_ap, in_ap, reads=(), writes=()):
        self.P.add(eng, lambda e: e.dma_start(out=out_ap, in_=in_ap), reads=reads, writes=writes, dma=True)

    def gbank(self):
        ring = self.gring
        b = ring[self.gi % len(ring)]
        self.gi += 1
        return self.PS[b]

    def psv(self, bank, off=0, n=512, p0=0, p1=128):
        return bank.v(F32, off, n, p0, p1)

    def r32(self, slot, n=512, off=0, p0=0, p1=128):
        return self.R32.v(F32, slot * 512 + off, n, p0, p1)

    def ctv(self, col, p0=0, p1=128):
        return self.CTB.v(F32, col, 1, p0, p1)

    def ptbuf(self, n=512):
        i = self.pti % 4
        self.pti += 1
        return self.PT.v(BF16, i * 512, n)

    def wload(self, dram_ap, n, slot=None):
        nslots = (n + 4095) // 4096
        if slot is not None:
            self.ring_next = slot
        if nslots == 2 and self.ring_next % 2 == 1:
            self.ring_next += 1
        slot = self.ring_next % 4
        assert slot + nslots <= 4
        self.ring_next += nslots
        base = slot * 4096
        v = self.WR.v(BF16, base, n)
        self.dma("pool", v.ap, dram_ap, writes=[v])
        return base

    def wv(self, base, off, n, p0=0, p1=128):
        return self.WR.v(BF16, base + off, n, p0, p1)

    def norm_stats(self, srcs, dn, rslot, n=512):
        bank = self.gbank()
        ps = self.psv(bank, 0, n)
        for i, s in enumerate(srcs):
            sq = self.SQ.v(BF16, (self.sqi % 2) * 512, n)
            self.sqi += 1
            self.act(sq, s, AF.Square)
            self.mm(ps, self.ones, sq, start=(i == 0), stop=(i == len(srcs) - 1))
        r = self.r32(rslot, n)
        self.act(r, ps, AF.Ln, scale=1.0 / dn, bias=EPS)
        self.act(r, r, AF.Exp, scale=-0.5)
        return r

    def xt(self, kc, g, n=512, off=0):
        return self.XT.v(F32, kc * SEQ + g * TG + off, n)

    def pre_norm(self, gcol, g, dst, rslot=0):
        rstd = self.norm_stats([self.xt(kc, g) for kc in range(NKC)], DM, rslot)
        for kc in range(NKC):
            self.stt(dst(kc), self.xt(kc, g), self.ctv(gcol + kc), rstd, ALU.mult, ALU.mult)

    def proj_post_norm(self, groups, emit_chain, gcol, banks):
        pend = None
        bi = 0
        for m in range(NKC):
            for gi_, (g, y, sbk, rs) in enumerate(groups):
                bank = self.PS[banks[bi % len(banks)]]
                bi += 1
                ps = self.psv(bank)
                emit_chain(m, gi_, ps)
                if pend is not None:
                    self.mm(self.psv(self.PS[pend[0]]), self.ones, pend[1], start=(pend[2] == 0), stop=(pend[2] == NKC - 1))
                self.copy(y(m), ps, eng="dve")
                sq = self.SQ.v(BF16, (self.sqi % 2) * 512, 512)
                self.sqi += 1
                self.act(sq, y(m), AF.Square)
                pend = (sbk, sq, m)
                if len(groups) > 1 and gi_ == 0:
                    pass
        self.mm(self.psv(self.PS[pend[0]]), self.ones, pend[1], start=(pend[2] == 0), stop=(pend[2] == NKC - 1))
        for (g, y, sbk, rs) in groups:
            r = self.r32(rs)
            self.act(r, self.psv(self.PS[sbk]), AF.Ln, scale=1.0 / DM, bias=EPS)
            self.act(r, r, AF.Exp, scale=-0.5)
            for m in range(NKC):
                self.tt(y(m), y(m), r, ALU.mult)
                self.stt(self.xt(m, g), y(m), self.ctv(gcol + m), self.xt(m, g), ALU.mult, ALU.add)

    def yview_ph(self, m):
        return self.PH.v(F32, m * 512, 512)

    def setup_consts(self):
        d = self.d
        ct = self.CTB.v(F32, 0, NCT)
        self.dma("sp", ct.ap, d["ct"], writes=[ct])
        self.dma("sp", self.ident.ap, d["ident"], writes=[self.ident])
        sel = self.SELB.v(F32, 0, 512)
        self.dma("sp", sel.ap, d["sel"], writes=[sel])
        self.P.add("dve", lambda e: e.memset(self.ones.ap, 1.0), writes=[self.ones])

    def load_x(self, s):
        x = self.d["x"]
        for g in range(NG):
            stage = self.PH.v(F32, 0, 4096)
            src = x[s, g * TG:(g + 1) * TG, :].rearrange("(t p) d -> p t d", p=128)
            self.dma("sp", stage.ap.rearrange("p (t d) -> p t d", t=4), src, writes=[stage])
            for kc in range(NKC):
                bank = self.gbank()
                for t in range(4):
                    self.tr(self.psv(bank, t * 128, 128), self.PH.v(F32, t * DM + kc * 128, 128), self.ident)
                self.copy(self.xt(kc, g), self.psv(bank))

    def store_out(self, s):
        for g in range(NG):
            stage = self.PH.v(F32, 0, 4096)
            for t in range(4):
                for half in range(2):
                    bank = self.gbank()
                    for c in range(4):
                        kc = half * 4 + c
                        self.tr(self.psv(bank, c * 128, 128), self.xt(kc, g, 128, t * 128), self.ident)
                    self.copy(self.PH.v(F32, t * DM + half * 512, 512), self.psv(bank))
            dst = self.out[s, g * TG:(g + 1) * TG, :].rearrange("(t p) d -> p t d", p=128)
            self.dma("sp", dst, stage.ap.rearrange("p (t d) -> p t d", t=4), reads=[stage])
            self.out_stage = stage

    def rope_tables(self, s):
        pos = self.d["pos"]
        tab = self.AUX.v(F32, 0, SEQ)
        tmpi = self.PH.v(I32, 0, SEQ)
        tmpf = self.PH.v(F32, 0, SEQ)
        tmp2 = self.PH.v(F32, SEQ, SEQ)
        tmp2i = self.PH.v(I32, SEQ, SEQ)
        self.dma("sp", tmpi.ap, pos[s, :].partition_broadcast(128), writes=[tmpi])
        self.P.add("dve", lambda e: e.tensor_copy(out=tab.ap, in_=tmpi.ap), reads=[tmpi], writes=[tab])
        self.ts(tab, tab, self.ctv(CT["invf"]), ALU.mult, self.ctv(CT["shift"]), ALU.add)
        self.ts(tmp2i, tab, 1.0 / (2.0 * math.pi), ALU.mult)
        self.P.add("dve", lambda e: e.tensor_copy(out=tmpf.ap, in_=tmp2i.ap), reads=[tmp2i], writes=[tmpf])
        C1 = 6.28125
        C2 = 2.0 * math.pi - C1
        self.stt(tab, tmpf, -C1, tab, ALU.mult, ALU.add)
        self.stt(tab, tmpf, -C2, tab, ALU.mult, ALU.add)
        self.ts(tmp2, tab, math.pi, ALU.is_gt)
        self.stt(tab, tmp2, -2.0 * math.pi, tab, ALU.mult, ALU.add)
        self.ts(tmp2, tab, -math.pi, ALU.is_lt)
        self.stt(tab, tmp2, 2.0 * math.pi, tab, ALU.mult, ALU.add)
        self.ts(tab, tab, math.pi, ALU.min, -math.pi, ALU.max)
        self.act(tab, tab, AF.Sin, scale=self.ctv(CT["sign"]))
        self.tab = tab

    def rope_apply(self, out, ps_r, ps_s, tok0, n, tslot):
        cos = self.AUX.v(F32, tok0, n, 0, 64)
        sin = self.AUX.v(F32, tok0, n, 64, 128)
        t1 = self.r32(tslot, n, 0, 0, 64)
        t2 = self.r32(tslot + 1, n, 0, 0, 64)
        self.tt(t1, ps_r, cos, ALU.mult)
        self.tt(t2, ps_s, sin, ALU.mult)
        self.tt(out, t1, t2, ALU.add)

    def attention(self, g, q_views, k_views, exp_fn, v_mm, finish, filler=None):
        ntile = 4 * g + 4

        def rest(kt, ps, col0, n, j):
            pt = self.ptbuf(n)
            exp_fn(ps, pt, kt, col0, n)
            if j >= 0:
                blk = T(pt.ap[:, 0:128], pt.cells)
                self.P.add("pool", lambda e, blk=blk: e.affine_select(
                    out=blk.ap, in_=blk.ap, pattern=[[1, 128]], compare_op=ALU.is_ge, fill=0.0,
                    base=0, channel_multiplier=-1), reads=[blk], writes=[blk])
            v_mm(kt, pt, col0, n, kt == 0, kt == ntile - 1)
            if filler is not None:
                filler.step()

        pend = None
        for kt in range(ntile):
            j = kt - 4 * g
            col0 = 128 * j if j > 0 else 0
            n = TG - col0
            sb = self.PS[kt % 2]
            ps = self.psv(sb, col0, n)
            ks = k_views(kt)
            qs = q_views(col0, n)
            for i in range(len(ks)):
                self.mm(ps, ks[i], qs[i], start=(i == 0), stop=(i == len(ks) - 1))
            if pend is not None:
                rest(*pend)
            pend = (kt, ps, col0, n, j)
        rest(*pend)
        finish()

    def mla(self, s, i):
        j = i // 2
        d = self.d
        A32 = lambda ch, tok0, n, p0=0, p1=128: self.BIG.v(BF16, ch * SEQ + tok0, n, p0, p1)
        OB = lambda h, tok0, n: self.BIG.v(BF16, 16384 + h * 1024 + tok0, n)
        Qn = lambda off, n: self.PH.v(BF16, off, n)
        Qr = lambda off, n: self.PH.v(BF16, 1024 + off, n, 0, 64)
        Kn = lambda off, n: self.PH.v(BF16, 2048 + off, n)
        Vh = lambda kt: self.PH.v(BF16, 4096 + kt * 128, 128)
        self.rope_tables(s)
        wa = self.wload(d["mla_wa"][j], 3072)
        wb = self.wload(d["mla_wb"][j], 3072)
        gpre = CT[("norm_mix_pre", i)]
        gq = CT[("mla_q_norm", j)]
        gkv = CT[("mla_kv_norm", j)]
        hTb = lambda b: (lambda kc: self.BIG.v(BF16, 16384 + b * 4096 + kc * 512, 512))
        self.pre_norm(gpre, 0, hTb(0), 0)
        for g in range(NG):
            hT = hTb(g % 2)
            c32 = lambda m: self.PH.v(F32, m * 512, 512)
            for m in range(5):
                bank = self.gbank()
                ps = self.psv(bank)
                for kc in range(NKC):
                    w = self.wv(wa, kc * 384 + m * 128, 128) if m < 3 else self.wv(wb, kc * 384 + (m - 3) * 128, 128)
                    self.mm(ps, w, hT(kc), start=(kc == 0), stop=(kc == NKC - 1))
                self.copy(c32(m), ps, eng="dve")
            b1 = self.gbank()
            b2 = self.gbank()
            pr = self.psv(b1, 0, TG, 0, 64)
            psw = self.psv(b2, 0, TG, 0, 64)
            for kc in range(NKC):
                self.mm(pr, self.wv(wb, kc * 384 + 256, 64), hT(kc), start=(kc == 0), stop=(kc == NKC - 1))
            for kc in range(NKC):
                self.mm(psw, self.wv(wb, kc * 384 + 320, 64), hT(kc), start=(kc == 0), stop=(kc == NKC - 1))
            if g + 1 < NG:
                self.pre_norm(gpre, g + 1, hTb((g + 1) % 2), 4 if (g + 1) % 2 else 0)
            self.rope_apply(A32(5, g * TG, TG, 0, 64), pr, psw, g * TG, TG, 2)
            rq = self.norm_stats([c32(m) for m in range(3)], 384, 1)
            for m in range(3):
                self.stt(A32(m, g * TG, TG), c32(m), self.ctv(gq + m), rq, ALU.mult, ALU.mult)
            rk = self.norm_stats([c32(3 + m) for m in range(2)], 256, 1)
            for m in range(2):
                self.stt(A32(3 + m, g * TG, TG), c32(3 + m), self.ctv(gkv + m), rk, ALU.mult, ALU.mult)
        wo = None
        scale = 192.0 ** -0.5
        BS = [
            dict(Qn=lambda off, n: self.PH.v(BF16, off, n),
                 Qr=lambda off, n: self.PH.v(BF16, 1024 + off, n, 0, 64),
                 Kn=lambda off, n: self.PH.v(BF16, 2048 + off, n),
                 V=lambda off, n: self.PH.v(BF16, 4096 + off, n)),
            dict(Qn=lambda off, n: self.PH.v(BF16, 6144 + off, n),
                 Qr=lambda off, n: self.PH.v(BF16, 7168 + off, n, 0, 64),
                 Kn=lambda off, n: self.BIG.v(BF16, 6 * SEQ + off, n),
                 V=lambda off, n: self.BIG.v(BF16, 7 * SEQ + off, n)),
        ]

        def gen_items(half, h, bs):
            nk = (half + 1) * 1024
            whb = self.wload(d["mla_wh"][j, h], 1280)
            wq = lambda kc, off, n: self.wv(whb, kc * 256 + off, n)
            wk = lambda kc: self.wv(whb, 768 + kc * 128, 128)
            wvv = lambda kc: self.wv(whb, 1024 + kc * 128, 128)
            items = []

            def k_item(kg):
                def f():
                    bank = self.gbank()
                    ps = self.psv(bank)
                    for kc in range(2):
                        self.mm(ps, wk(kc), A32(3 + kc, kg * TG, TG), start=(kc == 0), stop=(kc == 1))
                    yield
                    self.copy(bs["Kn"](kg * TG, TG), ps)
                return f

            def v_item(kq):
                def f():
                    bank = self.gbank()
                    for t in range(4):
                        kt = kq * 4 + t
                        ps = self.psv(bank, t * 128, 128)
                        for kc in range(2):
                            self.mm(ps, A32(3 + kc, kt * 128, 128), wvv(kc), start=(kc == 0), stop=(kc == 1))
                    yield
                    self.copy(bs["V"](kq * 512, 512), self.psv(bank))
                return f

            def qn_item(g2):
                def f():
                    g = 2 * half + g2
                    bank = self.gbank()
                    ps = self.psv(bank)
                    for kc in range(3):
                        self.mm(ps, wq(kc, 0, 128), A32(kc, g * TG, TG), start=(kc == 0), stop=(kc == 2))
                    yield
                    self.copy(bs["Qn"](g2 * TG, TG), ps)
                return f

            def qr_item(g2):
                def f():
                    g = 2 * half + g2
                    b1 = self.gbank()
                    b2 = self.gbank()
                    pr = self.psv(b1, 0, TG, 0, 64)
                    psw = self.psv(b2, 0, TG, 0, 64)
                    for kc in range(3):
                        self.mm(pr, wq(kc, 128, 64), A32(kc, g * TG, TG), start=(kc == 0), stop=(kc == 2))
                    for kc in range(3):
                        self.mm(psw, wq(kc, 192, 64), A32(kc, g * TG, TG), start=(kc == 0), stop=(kc == 2))
                    yield
                    self.rope_apply(bs["Qr"](g2 * TG, TG), pr, psw, g * TG, TG, 2)
                return f

            items.append(qn_item(0))
            items.append(qr_item(0))
            for kg in range(nk // TG):
                items.append(k_item(kg))
            for kq in range(nk // TG):
                items.append(v_item(kq))
            items.append(qn_item(1))
            items.append(qr_item(1))
            return items

        blk = 0
        filler = Filler()
        for half in range(2):
            self.gring = (6, 7)
            for it in gen_items(half, 0, BS[0]):
                filler.add(it)
            filler.drain()
            for h in range(8):
                bs = BS[h % 2]
                if h + 1 < 8:
                    for it in gen_items(half, h + 1, BS[(h + 1) % 2]):
                        filler.add(it)
                if h == 6:
                    wo = self.wload(d["mla_wo"][j], 8192)
                for g2 in range(2):
                    g = 2 * half + g2
                    ob = self.PS[2 + (blk % 2)]
                    dn = self.PS[4 + (blk % 2)]
                    blk += 1

                    def q_views(col0, n, g2=g2, bs=bs):
                        return [bs["Qn"](g2 * TG + col0, n), bs["Qr"](g2 * TG + col0, n)]

                    def k_views(kt, bs=bs):
                        return [bs["Kn"](kt * 128, 128), A32(5, kt * 128, 128, 0, 64)]

                    def exp_fn(ps, pt, kt, col0, n):
                        self.act(pt, ps, AF.Exp, scale=scale)

                    def v_mm(kt, pt, col0, n, first, last, ob=ob, dn=dn, bs=bs):
                        self.mm(self.psv(ob, col0, n), bs["V"](kt * 128, 128), pt, start=first, stop=last)
                        self.mm(self.psv(dn, col0, n), self.ones, pt, start=first, stop=last)

                    def finish(ob=ob, dn=dn, g2=g2, h=h):
                        rec = self.r32(4 + (h % 2))
                        self.recip(rec, self.psv(dn))
                        self.tt(OB(h, g2 * TG, TG), self.psv(ob), rec, ALU.mult)

                    self.attention(g, q_views, k_views, exp_fn, v_mm, finish, filler)
                filler.drain()
            self.gring = (5, 6, 7)
            gpost = CT[("norm_mix_post", i)]
            for g2 in range(2):
                g = 2 * half + g2
                def chain(m, gi_, ps, g2=g2, wo=wo):
                    for h in range(8):
                        self.mm(ps, self.wv(wo, h * 1024 + m * 128, 128), OB(h, g2 * TG, TG), start=(h == 0), stop=(h == 7))

                self.proj_post_norm([(g, self.yview_ph, 4, 0)], chain, gpost, (5, 6, 7, 0, 1))

    def mlstm(self, s, i):
        j = i // 2
        d = self.d
        A32 = lambda kc, tok0, n: self.BIG.v(BF16, kc * SEQ + tok0, n)
        OB = lambda c, tok0, n: self.BIG.v(BF16, 16384 + c * 1024 + tok0, n)
        Qn = lambda off, n: self.PH.v(BF16, off, n)
        Kn = lambda off, n: self.PH.v(BF16, 1024 + off, n)
        Vh = lambda kt, c: self.PH.v(BF16, 3072 + kt * 256 + c * 128, 128)
        gpre = CT[("norm_mix_pre", i)]
        GW = lambda tok0, n: self.AUX.v(F32, tok0, n, 0, 4)
        GU = lambda tok0, n: self.AUX.v(F32, tok0, n, 32, 36)
        GM = lambda tok0, n: self.AUX.v(F32, tok0, n, 64, 68)
        LI = lambda tok0, n: self.PH.v(F32, tok0, n, 0, 4)
        LF = lambda tok0, n: self.PH.v(F32, SEQ + tok0, n, 0, 4)
        wg = self.WGB.v(BF16, 0, 64)
        self.dma("pool", wg.ap, d["ml_wg"][j], writes=[wg])
        bcol = CT[("mlstm_b", j)]
        for g in range(NG):
            self.pre_norm(gpre, g, lambda kc: A32(kc, g * TG, TG))
            bi = self.gbank()
            bf = self.gbank()
            pi = self.psv(bi, 0, TG, 0, 4)
            pf = self.psv(bf, 0, TG, 0, 4)
            for kc in range(NKC):
                self.mm(pi, self.WGB.v(BF16, kc * 8, 4), A32(kc, g * TG, TG), start=(kc == 0), stop=(kc == NKC - 1))
            for kc in range(NKC):
                self.mm(pf, self.WGB.v(BF16, kc * 8 + 4, 4), A32(kc, g * TG, TG), start=(kc == 0), stop=(kc == NKC - 1))
            self.act(LI(g * TG, TG), pi, AF.Identity, bias=self.ctv(bcol, 0, 4))
            self.act(LF(g * TG, TG), pf, AF.Identity, bias=self.ctv(bcol + 1, 0, 4))
        li = LI(0, SEQ)
        lf = LF(0, SEQ)
        gu = GU(0, SEQ)
        gw = GW(0, SEQ)
        gm = GM(0, SEQ)
        self.act(lf, lf, AF.Exp, scale=-1.0)
        self.act(lf, lf, AF.Ln, bias=1.0)
        self.ts(lf, lf, -1.0, ALU.mult)
        mtmp = GW(0, SEQ)
        self.P.add("dve", lambda e: e.tensor_tensor_scan(out=mtmp.ap, data0=lf.ap, data1=li.ap, initial=0.0,
                                                         op0=ALU.add, op1=ALU.max), reads=[lf, li], writes=[mtmp])
        self.P.add("dve", lambda e: e.tensor_copy(out=gm.ap, in_=mtmp.ap), reads=[mtmp], writes=[gm])
        one = self.SMB.v(F32, 0, 1, 0, 4)
        self.P.add("dve", lambda e: e.memset(one.ap, 1.0), writes=[one])
        gb = self.R32.v(F32, 0, SEQ, 0, 4)
        self.P.add("dve", lambda e: e.tensor_tensor_scan(out=gb.ap, data0=one.ap.to_broadcast([4, SEQ]), data1=lf.ap,
                                                         initial=0.0, op0=ALU.mult, op1=ALU.add),
                   reads=[lf, one], writes=[gb])
        self.tt(gu, gb, mtmp, ALU.subtract)
        self.tt(gw, li, gb, ALU.subtract)
        bank = self.gbank()
        for kt in range(16):
            self.mm(self.psv(bank, kt * 4, 4), GW(kt * 128, 128), self.IDB.v(F32, 0, 4, 0, 4), start=True, stop=True)
        wt = self.WTB.v(F32, 0, 64)
        self.copy(wt, self.psv(bank, 0, 64), eng="dve")
        wo = None
        hn = CT[("mlstm_head_norm", j)]
        fl = Filler()
        blk = 0

        def bcast_item(h, g, su, se):
            def f():
                b1 = self.gbank()
                self.mm(self.psv(b1), self.SELB.v(F32, h * 128, 128, 32, 36), GU(g * TG, TG), start=True, stop=True)
                b2 = self.gbank()
                self.mm(self.psv(b2), self.SELB.v(F32, h * 128, 128, 64, 68), GM(g * TG, TG), start=True, stop=True)
                yield
                self.copy(self.r32(su), self.psv(b1), eng="dve")
                self.act(self.r32(se), self.psv(b2), AF.Exp, scale=-1.0)
            return f

        whb_next = self.wload(d["ml_wh"][j, 0], 6144, slot=0)
        for half in range(2):
            nk = (half + 1) * 1024
            for h in range(4):
                whb = whb_next
                wcol = lambda kc, off, n, whb=whb: self.wv(whb, kc * 768 + off, n)

                def k_item(kg, wcol=wcol):
                    def f():
                        bank = self.gbank()
                        ps = self.psv(bank)
                        for kc in range(NKC):
                            self.mm(ps, wcol(kc, 128, 128), A32(kc, kg * TG, TG), start=(kc == 0), stop=(kc == NKC - 1))
                        yield
                        self.copy(Kn(kg * TG, TG), ps)
                    return f

                def v_item(kp, wcol=wcol):
                    def f():
                        bank = self.gbank()
                        for t in range(2):
                            kt = kp * 2 + t
                            ps = self.psv(bank, t * 256, 256)
                            for kc in range(NKC):
                                self.mm(ps, A32(kc, kt * 128, 128), wcol(kc, 256, 256), start=(kc == 0), stop=(kc == NKC - 1))
                        yield
                        self.copy(self.PH.v(BF16, 3072 + kp * 512, 512), self.psv(bank))
                    return f

                def q_item(g2, wcol=wcol, half=half):
                    def f():
                        g = 2 * half + g2
                        bank = self.gbank()
                        ps = self.psv(bank)
                        for kc in range(NKC):
                            self.mm(ps, wcol(kc, 0, 128), A32(kc, g * TG, TG), start=(kc == 0), stop=(kc == NKC - 1))
                        yield
                        self.copy(Qn(g2 * TG, TG), ps, scale=128.0 ** -0.5)
                    return f

                su0, se0 = (4, 8)[blk % 2], (3, 9)[blk % 2]
                epi = fl.queue
                fl.queue = []
                fl.add(bcast_item(h, 2 * half, su0, se0))
                fl.add(q_item(0))
                for kg in range(nk // TG):
                    fl.add(k_item(kg))
                for kp in range(nk // 256):
                    fl.add(v_item(kp))
                fl.add(q_item(1))
                genq = fl.queue
                merged = []
                for ii in range(max(len(genq), len(epi))):
                    if ii < len(genq):
                        merged.append(genq[ii])
                    if ii < len(epi):
                        merged.append(epi[ii])
                fl.queue = merged
                fl.drain()
                if h < 3:
                    whb_next = self.wload(d["ml_wh"][j, h + 1], 6144)
                else:
                    wo = self.wload(d["ml_wo"][j], 8192)
                for g2 in range(2):
                    g = 2 * half + g2
                    su, se = (4, 8)[blk % 2], (3, 9)[blk % 2]
                    blk += 1
                    ubc = self.r32(su)
                    enm = self.r32(se)
                    n0 = self.PS[2]
                    n1 = self.PS[3]
                    dn = self.PS[4]
                    if g2 == 0:
                        fl.add(bcast_item(h, g + 1, (4, 8)[blk % 2], (3, 9)[blk % 2]))

                    def q_views(col0, n, g2=g2):
                        return [Qn(g2 * TG + col0, n)]

                    def k_views(kt):
                        return [Kn(kt * 128, 128)]

                    def exp_fn(ps, pt, kt, col0, n, h=h, ubc=ubc):
                        dsl = 5 + (kt % 2)
                        dt_ = self.r32(dsl, n)
                        ub = T(ubc.ap[:, col0:col0 + n], ubc.cells)
                        self.act(dt_, ub, AF.Exp, bias=self.WTB.v(F32, kt * 4 + h, 1))
                        self.tt(pt, ps, dt_, ALU.mult)

                    def v_mm(kt, pt, col0, n, first, last):
                        self.mm(self.psv(n0, col0, n), Vh(kt, 0), pt, start=first, stop=last)
                        self.mm(self.psv(n1, col0, n), Vh(kt, 1), pt, start=first, stop=last)
                        self.mm(self.psv(dn, col0, n), self.ones, pt, start=first, stop=last)

                    def finish(h=h, g=g, g2=g2, enm=enm, whb=whb):
                        fl.drain()
                        rec = self.r32(2)
                        hs = [self.r32(0), self.r32(1)]
                        self.copy(hs[0], self.psv(n0), eng="act")
                        self.copy(hs[1], self.psv(n1), eng="dve")
                        self.act(rec, self.psv(dn), AF.Abs)
                        sqs = [self.SQ.v(BF16, c * 512, 512) for c in range(2)]

                        def norm_item():
                            self.tt(rec, rec, enm, ALU.max)
                            self.recip(rec, rec)
                            self.tt(hs[0], hs[0], rec, ALU.mult)
                            self.tt(hs[1], hs[1], rec, ALU.mult)
                            yield
                            for c in range(2):
                                self.act(sqs[c], hs[c], AF.Square)

                        def stats_item():
                            yield
                            bank = self.gbank()
                            ps = self.psv(bank)
                            for c in range(2):
                                self.mm(ps, self.ones, sqs[c], start=(c == 0), stop=(c == 1))
                            yield
                            rh = self.r32(2)
                            self.act(rh, ps, AF.Ln, scale=1.0 / 256, bias=EPS)
                            self.act(rh, rh, AF.Exp, scale=-0.5)

                        def og_item(c):
                            def f():
                                rh = self.r32(2)
                                bank = self.gbank()
                                ps = self.psv(bank)
                                for kc in range(NKC):
                                    self.mm(ps, self.wv(whb, kc * 768 + 512 + c * 128, 128), A32(kc, g * TG, TG),
                                            start=(kc == 0), stop=(kc == NKC - 1))
                                yield
                                e = self.r32(7 if c == 0 else 10)
                                self.act(e, ps, AF.Exp, scale=-1.0)
                                self.act(e, e, AF.Ln, bias=1.0)
                                self.act(e, e, AF.Exp, scale=-1.0)
                                yield
                                self.stt(hs[c], hs[c], self.ctv(hn + h * 2 + c), rh, ALU.mult, ALU.mult)
                                self.tt(OB(h * 2 + c, g2 * TG, TG), hs[c], e, ALU.mult)
                            return f

                        fl.add(norm_item)
                        fl.add(stats_item)
                        fl.add(og_item(0))
                        fl.add(og_item(1))

                    self.attention(g, q_views, k_views, exp_fn, v_mm, finish, fl)
            fl.drain()
            if half == 0:
                whb_next = self.wload(d["ml_wh"][j, 0], 6144)
            gpost = CT[("norm_mix_post", i)]
            for g2 in range(2):
                g = 2 * half + g2
                def chain(m, gi_, ps, g2=g2, wo=wo):
                    for kc in range(NKC):
                        self.mm(ps, self.wv(wo, kc * 1024 + m * 128, 128), OB(kc, g2 * TG, TG), start=(kc == 0), stop=(kc == NKC - 1))

                self.proj_post_norm([(g, self.yview_ph, 4, 0)], chain, gpost, (5, 6, 7, 0, 1))
            if half == 0:
                wo = None

    def memx(self, s, i):
        d = self.d
        KM = lambda m, off, n: self.AUX.v(BF16, m * 256 + off, n)
        VM = lambda kt, off, n: self.AUX.v(BF16, 2048 + kt * 1024 + off, n)
        MT = lambda kc, off, n: self.PH.v(BF16, 4096 + kc * 256 + off, n)
        mtok = self.PH.v(F32, 0, 2048)
        self.dma("sp", mtok.ap.rearrange("p (t d) -> p t d", t=2),
                 d["mem"][s].rearrange("(t p) d -> p t d", p=128), writes=[mtok])
        ss = self.SMB.v(F32, 8, 2)
        junk = self.r32(7)
        for t in range(2):
            src = self.PH.v(F32, t * 1024, 512)
            src2 = self.PH.v(F32, t * 1024 + 512, 512)
            s1 = self.SMB.v(F32, 16 + 2 * t, 1)
            s2 = self.SMB.v(F32, 17 + 2 * t, 1)
            self.P.add("act", lambda e, src=src, s1=s1: e.activation(out=junk.ap, in_=src.ap, func=AF.Square, accum_out=s1.ap),
                       reads=[src], writes=[junk, s1])
            self.P.add("act", lambda e, src2=src2, s2=s2: e.activation(out=junk.ap, in_=src2.ap, func=AF.Square, accum_out=s2.ap),
                       reads=[src2], writes=[junk, s2])
            self.tt(self.SMB.v(F32, 8 + t, 1), s1, s2, ALU.add)
        self.act(ss, ss, AF.Ln, scale=1.0 / DM, bias=EPS)
        self.act(ss, ss, AF.Exp, scale=-0.5)
        for t in range(2):
            mt_ = self.PH.v(F32, t * 1024, 1024)
            self.ts(mt_, mt_, self.SMB.v(F32, 8 + t, 1), ALU.mult)
        gkv = CT[("norm_mem_kv", i)]
        for kc in range(NKC):
            bank = self.gbank()
            for t in range(2):
                self.tr(self.psv(bank, t * 128, 128), self.PH.v(F32, t * 1024 + kc * 128, 128), self.ident)
            self.ts(MT(kc, 0, 256), self.psv(bank, 0, 256), self.ctv(gkv + kc), ALU.mult)
        for piece in range(4):
            wb = self.wload(d["mm_wkv"][i, piece], 4096)
            if piece < 2:
                for mm_ in range(4):
                    m = piece * 4 + mm_
                    bank = self.gbank()
                    ps = self.psv(bank, 0, 256)
                    for kc in range(NKC):
                        self.mm(ps, self.wv(wb, kc * 512 + mm_ * 128, 128), MT(kc, 0, 256), start=(kc == 0), stop=(kc == NKC - 1))
                    self.copy(KM(m, 0, 256), ps)
            else:
                nh = piece - 2
                for kt in range(2):
                    bank = self.gbank()
                    ps = self.psv(bank)
                    for kc in range(NKC):
                        self.mm(ps, MT(kc, kt * 128, 128), self.wv(wb, kc * 512, 512), start=(kc == 0), stop=(kc == NKC - 1))
                    self.copy(VM(kt, nh * 512, 512), ps)
        wq = self.wload(d["mm_wq"][i], 8192)
        wo = self.wload(d["mm_wo"][i], 8192)
        gq = CT[("norm_mem_q", i)]
        gpost = CT[("norm_mem_post", i)]
        hTb = lambda b: (lambda kc: self.BIG.v(BF16, b * 12288 + kc * 512, 512))
        Qg = lambda m: self.BIG.v(BF16, 4096 + m * 512, 512)
        Og = lambda m: self.BIG.v(BF16, 8192 + m * 512, 512)
        self.pre_norm(gq, 0, hTb(0), 0)
        for g in range(NG):
            hT = hTb(g % 2)
            for m in range(NKC):
                bank = self.gbank()
                ps = self.psv(bank)
                for kc in range(NKC):
                    self.mm(ps, self.wv(wq, kc * 1024 + m * 128, 128), hT(kc), start=(kc == 0), stop=(kc == NKC - 1))
                self.copy(Qg(m), ps)
            if g + 1 < NG:
                self.pre_norm(gq, g + 1, hTb((g + 1) % 2), (g + 1) % 2)
            def pv(h, pts):
                o0, o1, dn = (self.PS[2], self.PS[3], self.PS[4]) if h % 2 == 0 else (self.PS[5], self.PS[6], self.PS[7])
                for kt in range(2):
                    self.mm(self.psv(o0), VM(kt, h * 256, 128), pts[kt], start=(kt == 0), stop=(kt == 1))
                for kt in range(2):
                    self.mm(self.psv(o1), VM(kt, h * 256 + 128, 128), pts[kt], start=(kt == 0), stop=(kt == 1))
                for kt in range(2):
                    self.mm(self.psv(dn), self.ones, pts[kt], start=(kt == 0), stop=(kt == 1))
                rec = self.r32(4)
                self.recip(rec, self.psv(dn))
                self.tt(Og(2 * h), self.psv(o0), rec, ALU.mult)
                self.tt(Og(2 * h + 1), self.psv(o1), rec, ALU.mult)

            pend = None
            for h in range(4):
                pts = []
                for kt in range(2):
                    sb = self.PS[kt % 2]
                    ps = self.psv(sb)
                    for c in range(2):
                        self.mm(ps, KM(2 * h + c, kt * 128, 128), Qg(2 * h + c), start=(c == 0), stop=(c == 1))
                    pt = self.ptbuf()
                    self.act(pt, ps, AF.Exp, scale=1.0 / 16.0)
                    pts.append(pt)
                if pend is not None:
                    pv(*pend)
                pend = (h, pts)
            pv(*pend)

            def chain(m, gi_, ps):
                for kc in range(NKC):
                    self.mm(ps, self.wv(wo, kc * 1024 + m * 128, 128), Og(kc), start=(kc == 0), stop=(kc == NKC - 1))

            self.proj_post_norm([(g, self.yview_ph, 4, 7)], chain, gpost, (5, 6, 7))

    def ffn(self, s, i):
        d = self.d
        gpre = CT[("norm_ffn_pre", i)]
        gpost = CT[("norm_ffn_post", i)]
        hT = lambda kc, g2: self.PH.v(BF16, kc * 1024 + g2 * 512, 512)
        AT = lambda jj, g2: self.BIG.v(BF16, jj * 1024 + g2 * 512, 512)

        def y1(m):
            if m < 6:
                return self.AUX.v(F32, m * 512, 512)
            return self.BIG.v(F32, 11264 + (m - 6) * 512, 512)

        for tg in range(2):
            for g2 in range(2):
                self.pre_norm(gpre, 2 * tg + g2, lambda kc: hT(kc, g2))
            loads = {}
            for p in range(min(2, 11)):
                loads[p] = self.wload(d["ff_gu"][i, p], 4096)
            dn_loads = {}
            for p in range(11):
                if p + 2 < 11:
                    loads[p + 2] = self.wload(d["ff_gu"][i, p + 2], 4096)
                elif p + 2 - 11 < 8:
                    dn_loads[p + 2 - 11] = self.wload(d["ff_dn"][i, p + 2 - 11], 2816)
                wb = loads[p]
                for jj in range(2):
                    for g2 in range(2):
                        bg = self.PS[(self.gi) % 8]
                        self.gi += 1
                        bu = self.PS[(self.gi) % 8]
                        self.gi += 1
                        pg = self.psv(bg)
                        pu = self.psv(bu)
                        for kc in range(NKC):
                            self.mm(pg, self.wv(wb, kc * 512 + jj * 128, 128), hT(kc, g2), start=(kc == 0), stop=(kc == NKC - 1))
                        for kc in range(NKC):
                            self.mm(pu, self.wv(wb, kc * 512 + 256 + jj * 128, 128), hT(kc, g2), start=(kc == 0), stop=(kc == NKC - 1))
                        sg = self.r32(self.sqi % 8)
                        self.sqi += 1
                        self.act(sg, pg, AF.Silu)
                        self.tt(AT(2 * p + jj, g2), sg, pu, ALU.mult)
            def chain(m, gi_, ps, tg=tg):
                if gi_ == 0 and m + 2 < 8:
                    dn_loads[m + 2] = self.wload(d["ff_dn"][i, m + 2], 2816)
                wb = dn_loads[m]
                for jj in range(22):
                    self.mm(ps, self.wv(wb, jj * 128, 128), AT(jj, gi_), start=(jj == 0), stop=(jj == 21))

            self.proj_post_norm([(2 * tg, self.yview_ph, 4, 0), (2 * tg + 1, y1, 3, 1)], chain, gpost, (0, 1, 2, 5, 6, 7))

    def build(self):
        self.declare()
        with ExitStack() as st:
            self.alloc(st)
            self.setup_consts()
            done = False
            for s in range(self.nseq):
                self.load_x(s)
                for i in range(self.layers[0], self.layers[1]):
                    for sub in range(3):
                        if sub == 0:
                            if i % 2 == 0:
                                self.mla(s, i)
                            else:
                                self.mlstm(s, i)
                        elif sub == 1:
                            self.memx(s, i)
                        else:
                            self.ffn(s, i)
                        if self.stop is not None and (i, sub) == tuple(self.stop):
                            done = True
                            break
                    if done:
                        break
                done = False
                self.store_out(s)
            self.P.add("sp", None, writes=[self.out_stage])
            self.stats = self.P.finalize()
            self.P.emit(self.nc)
        return self.nc


def _kc(w):
    k, n = w.shape
    return np.ascontiguousarray(w.reshape(k // 128, 128, n).transpose(1, 0, 2)).reshape(128, (k // 128) * n)


def host_consts():
    ct = np.zeros((128, NCT), np.float32)
    inv_freq = (10000.0 ** (-np.arange(0, 64, 2, dtype=np.float32) / np.float32(64))).astype(np.float32)
    p = np.arange(128)
    ct[:, CT["invf"]] = inv_freq[p % 32]
    ct[:, CT["shift"]] = np.where(p < 64, np.float32(math.pi / 2), np.float32(0.0))
    ct[:, CT["sign"]] = np.where((p >= 64) & (p < 96), np.float32(-1.0), np.float32(1.0))
    ident = np.eye(128, dtype=np.float32)
    sel = np.zeros((128, 4, 128), np.float32)
    for q in range(4):
        for h in range(4):
            sel[q * 32 + h, h, :] = 1.0
    return ct, ident, sel.reshape(128, 512)


def host_layout(inp):
    f = lambda k: np.asarray(inp[k], dtype=np.float32)
    ct, ident, sel = host_consts()
    for i in range(DEPTH):
        for n in NORM6 + ("norm_mem_kv",):
            ct[:, CT[(n, i)]:CT[(n, i)] + 8] = f(n)[i].reshape(8, 128).T
    for j in range(2):
        ct[:, CT[("mla_q_norm", j)]:CT[("mla_q_norm", j)] + 3] = f("mla_q_norm")[j].reshape(3, 128).T
        ct[:, CT[("mla_kv_norm", j)]:CT[("mla_kv_norm", j)] + 2] = f("mla_kv_norm")[j].reshape(2, 128).T
        hn = f("mlstm_head_norm")[j]
        for h in range(4):
            ct[:, CT[("mlstm_head_norm", j)] + 2 * h:CT[("mlstm_head_norm", j)] + 2 * h + 2] = hn[h].reshape(2, 128).T
        bg = f("mlstm_b_gates")[j]
        ct[0:4, CT[("mlstm_b", j)]] = bg[0:4]
        ct[0:4, CT[("mlstm_b", j)] + 1] = bg[4:8]
    w = {"ct": ct, "ident": ident, "sel": sel}
    w_in = f("mla_w_in")
    w_uq = f("mla_w_uq")
    w_ukv = f("mla_w_ukv")
    w_o = f("mla_w_o")
    wa = np.zeros((2, 128, 3072), np.float32)
    wb = np.zeros((2, 128, 3072), np.float32)
    wh = np.zeros((2, 8, 128, 1280), np.float32)
    wo = np.zeros((2, 128, 8192), np.float32)
    for j in range(2):
        wa[j] = _kc(w_in[j][:, 0:384])
        kr = w_in[j][:, 640:704]
        wb[j] = _kc(np.concatenate([w_in[j][:, 384:640], kr, kr[:, 32:64], kr[:, 0:32]], axis=1))
        for h in range(8):
            qn = w_uq[j][:, h * 192:h * 192 + 128]
            qr = w_uq[j][:, h * 192 + 128:h * 192 + 192]
            wq = _kc(np.concatenate([qn, qr, qr[:, 32:64], qr[:, 0:32]], axis=1))
            wk = _kc(w_ukv[j][:, h * 256:h * 256 + 128])
            wv = _kc(w_ukv[j][:, h * 256 + 128:h * 256 + 256])
            wh[j, h] = np.concatenate([wq, wk, wv], axis=1)
        wo[j] = _kc(w_o[j])
    w.update(mla_wa=wa, mla_wb=wb, mla_wh=wh, mla_wo=wo)
    m_in = f("mlstm_w_in")
    m_o = f("mlstm_w_o")
    mh = np.zeros((2, 4, 128, 6144), np.float32)
    mg = np.zeros((2, 128, 64), np.float32)
    mo = np.zeros((2, 128, 8192), np.float32)
    for j in range(2):
        for h in range(4):
            cols = np.concatenate([m_in[j][:, h * 128:(h + 1) * 128],
                                   m_in[j][:, 512 + h * 128:512 + (h + 1) * 128],
                                   m_in[j][:, 1024 + h * 256:1024 + (h + 1) * 256],
                                   m_in[j][:, 2048 + h * 256:2048 + (h + 1) * 256]], axis=1)
            mh[j, h] = _kc(cols)
        mg[j] = _kc(m_in[j][:, 3072:3080])
        mo[j] = _kc(m_o[j])
    w.update(ml_wh=mh, ml_wg=mg, ml_wo=mo)
    wq_ = f("mem_w_q")
    wkv_ = f("mem_w_kv")
    wo_ = f("mem_w_o")
    mq = np.zeros((4, 128, 8192), np.float32)
    mkv = np.zeros((4, 4, 128, 4096), np.float32)
    mo2 = np.zeros((4, 128, 8192), np.float32)
    for i in range(DEPTH):
        mq[i] = _kc(wq_[i])
        for p in range(4):
            mkv[i, p] = _kc(wkv_[i][:, p * 512:(p + 1) * 512])
        mo2[i] = _kc(wo_[i])
    w.update(mm_wq=mq, mm_wkv=mkv, mm_wo=mo2)
    gu_ = f("ffn_w_gate_up")
    dn_ = f("ffn_w_down")
    gu = np.zeros((4, 11, 128, 4096), np.float32)
    dn = np.zeros((4, 8, 128, 2816), np.float32)
    for i in range(DEPTH):
        for p in range(11):
            gu[i, p] = _kc(np.concatenate([gu_[i][:, p * 256:(p + 1) * 256],
                                           gu_[i][:, DFF + p * 256:DFF + (p + 1) * 256]], axis=1))
        for m in range(8):
            dn[i, m] = _kc(dn_[i][:, m * 128:(m + 1) * 128])
    w.update(ff_gu=gu, ff_dn=dn)
    return w


_NC_CACHE = {}


def kernel(**inputs):
    w = host_layout(inputs)
    x = np.asarray(inputs["x"], np.float32)
    mem = np.asarray(inputs["mem"], np.float32)
    pos = np.asarray(inputs["positions"], np.int32)
    if "nc" not in _NC_CACHE:
        _NC_CACHE["nc"] = MK(nseq=2).build()
    nc = _NC_CACHE["nc"]
    in_maps = []
    for c in range(NCORES):
        m = dict(w)
        m["x"] = np.ascontiguousarray(x[2 * c:2 * c + 2])
        m["mem"] = np.ascontiguousarray(mem[2 * c:2 * c + 2])
        m["pos"] = np.ascontiguousarray(pos[2 * c:2 * c + 2])
        in_maps.append(m)
    res = run_bass_kernel_spmd(nc, in_maps, core_ids=list(range(NCORES)))
    return np.concatenate([r["out"] for r in res.results], axis=0)
```
